# Optimizing a Trainium2 kernel written in Bass

```python
import jax, jax.numpy as jnp
from jax import lax
import numpy as np

D_MODEL = 1024
BATCH = 8
SEQ = 4096
DEPTH = 2

CHUNK = 64
MEM_LEN = 256
D_CONV = 3 * D_MODEL // 8
CONV_WIDTH = 31
D_SGU = 3 * D_MODEL // 8
SGU_HEADS = 4
SGU_CHUNK = 128
D_POOL = D_MODEL // 4
POOL_WINDOWS = (2, 4, 8, 16)
POOL_GROUPS = len(POOL_WINDOWS)
D_MIX = D_CONV + D_SGU + D_POOL
D_IN = 2 * D_CONV + 2 * D_SGU + D_POOL
X_HEADS = 4
X_HEAD_DIM = D_MODEL // X_HEADS
D_FF = ((8 * D_MODEL // 3 + 127) // 128) * 128
N_EXPERTS = 8
TOP_K = 2
D_FF_EXPERT = D_FF
N_DENSE = (DEPTH + 1) // 2
N_MOE = DEPTH // 2
EPS = 1e-6

kernel_name = 'hybrid_conv_sgu_pool_moe_block'


def rms_norm(x, g):
    xf = x.astype(jnp.float32)
    y = xf * lax.rsqrt(jnp.mean(xf * xf, axis=-1, keepdims=True) + EPS)
    return (y * g.astype(jnp.float32)).astype(x.dtype)


def layer_norm(x, g, b):
    xf = x.astype(jnp.float32)
    mu = jnp.mean(xf, axis=-1, keepdims=True)
    var = jnp.mean(jnp.square(xf - mu), axis=-1, keepdims=True)
    y = (xf - mu) * lax.rsqrt(var + EPS) * g.astype(jnp.float32) + b.astype(jnp.float32)
    return y.astype(x.dtype)


def conformer_conv(a, conv_w, conv_b, ln_g, ln_b):
    h = a[..., :D_CONV] * jax.nn.sigmoid(a[..., D_CONV:])
    h = lax.conv_general_dilated(
        h, conv_w[:, None, :], window_strides=(1,),
        padding=[(CONV_WIDTH - 1, 0)],
        dimension_numbers=('NWC', 'WIO', 'NWC'),
        feature_group_count=D_CONV) + conv_b
    h = layer_norm(h, ln_g, ln_b)
    return jax.nn.silu(h)


def spatial_gating(z, ln_g, ln_b, sgu_w, sgu_b):
    z = jax.nn.gelu(z)
    u, v = z[..., :D_SGU], z[..., D_SGU:]
    v = layer_norm(v, ln_g, ln_b)
    B, S, _ = v.shape
    v = v.reshape(B, S // SGU_CHUNK, SGU_CHUNK, SGU_HEADS, D_SGU // SGU_HEADS)
    blk = jnp.arange(SGU_CHUNK) // CHUNK
    mask = blk[:, None] >= blk[None, :]
    w = jnp.where(mask[None], sgu_w, 0)
    s = jnp.einsum('hij,bcjhd->bcihd', w, v) + sgu_b.T[None, None, :, :, None]
    return u * s.reshape(B, S, D_SGU)


def multiscale_pool(c, pool_w, pool_b, pool_scale):
    B, S, _ = c.shape
    gd = D_POOL // POOL_GROUPS
    cf = c.astype(jnp.float32)
    pos = jnp.arange(S)
    outs = []
    for gi, w in enumerate(POOL_WINDOWS):
        cg = cf[..., gi * gd:(gi + 1) * gd]
        cs = jnp.cumsum(cg, axis=1)
        prev = jnp.pad(cs, ((0, 0), (w, 0), (0, 0)))[:, :S]
        cnt = jnp.minimum(pos + 1, w).astype(jnp.float32)[None, :, None]
        outs.append((cs - prev) / cnt - cg)
    p = jnp.stack(outs, axis=2).astype(c.dtype)
    y = jnp.einsum('bsgi,gio->bsgo', p, pool_w) + pool_b
    return y.reshape(B, S, D_POOL) * pool_scale


def memory_cross_attention(h, m, wq, wk, wv, wo):
    B, S, D = h.shape
    M = m.shape[1]
    q = (h @ wq).reshape(B, S, X_HEADS, X_HEAD_DIM)
    k = (m @ wk).reshape(B, M, X_HEADS, X_HEAD_DIM)
    v = (m @ wv).reshape(B, M, X_HEADS, X_HEAD_DIM)
    s = jnp.einsum('bshd,bmhd->bhsm', q, k).astype(jnp.float32) * (X_HEAD_DIM ** -0.5)
    p = jax.nn.softmax(s, axis=-1).astype(h.dtype)
    o = jnp.einsum('bhsm,bmhd->bshd', p, v).reshape(B, S, D)
    return o @ wo


def swiglu(h, wg, wu, wd):
    return (jax.nn.silu(h @ wg) * (h @ wu)) @ wd


def moe_swiglu(h, router_w, wg, wu, wd):
    B, S, D = h.shape
    t = h.reshape(B * S, D)
    logits = (t @ router_w).astype(jnp.float32)
    top_v, top_i = lax.top_k(logits, TOP_K)
    gates = jax.nn.softmax(top_v, axis=-1)
    combine = jnp.sum(jax.nn.one_hot(top_i, N_EXPERTS, dtype=jnp.float32) * gates[..., None], axis=1)
    combine = combine.astype(h.dtype)
    y = jnp.zeros_like(t)
    for e in range(N_EXPERTS):
        y = y + combine[:, e:e + 1] * swiglu(t, wg[e], wu[e], wd[e])
    return y.reshape(B, S, D)


def setup_inputs(seed: int = 0) -> dict:
    key = jax.random.key(seed)
    keys = jax.random.split(key, 32)
    cnt = [0]
    f32 = jnp.float32

    def nk():
        k = keys[cnt[0]]
        cnt[0] += 1
        return k

    def nrm(shape, scale):
        return jax.random.normal(nk(), shape, f32) * scale

    def gain(shape):
        return 1.0 + nrm(shape, 0.02)

    L, D = DEPTH, D_MODEL
    gd = D_POOL // POOL_GROUPS
    return {
        'x': nrm((BATCH, SEQ, D), 1.0),
        'mem': nrm((BATCH, MEM_LEN, D), 1.0),
        'mix_norm_g': gain((L, D)),
        'w_in': nrm((L, D, D_IN), D ** -0.5),
        'conv_w': nrm((L, CONV_WIDTH, D_CONV), CONV_WIDTH ** -0.5),
        'conv_b': nrm((L, D_CONV), 0.02),
        'conv_ln_g': gain((L, D_CONV)),
        'conv_ln_b': nrm((L, D_CONV), 0.02),
        'sgu_ln_g': gain((L, D_SGU)),
        'sgu_ln_b': nrm((L, D_SGU), 0.02),
        'sgu_w': nrm((L, SGU_HEADS, SGU_CHUNK, SGU_CHUNK), SGU_CHUNK ** -0.5),
        'sgu_b': 1.0 + nrm((L, SGU_HEADS, SGU_CHUNK), 0.1),
        'pool_w': nrm((L, POOL_GROUPS, gd, gd), gd ** -0.5),
        'pool_b': nrm((L, POOL_GROUPS, gd), 0.02),
        'pool_scale': 1.0 + nrm((L, D_POOL), 0.05),
        'w_out': nrm((L, D_MIX, D), D_MIX ** -0.5),
        'xattn_norm_g': gain((L, D)),
        'mem_norm_g': gain((L, D)),
        'xattn_wq': nrm((L, D, D), D ** -0.5),
        'xattn_wk': nrm((L, D, D), D ** -0.5),
        'xattn_wv': nrm((L, D, D), D ** -0.5),
        'xattn_wo': nrm((L, D, D), D ** -0.5),
        'ffn_norm_g': gain((L, D)),
        'ffn_wg': nrm((N_DENSE, D, D_FF), D ** -0.5),
        'ffn_wu': nrm((N_DENSE, D, D_FF), D ** -0.5),
        'ffn_wd': nrm((N_DENSE, D_FF, D), D_FF ** -0.5),
        'router_w': nrm((N_MOE, D, N_EXPERTS), D ** -0.5),
        'moe_wg': nrm((N_MOE, N_EXPERTS, D, D_FF_EXPERT), D ** -0.5),
        'moe_wu': nrm((N_MOE, N_EXPERTS, D, D_FF_EXPERT), D ** -0.5),
        'moe_wd': nrm((N_MOE, N_EXPERTS, D_FF_EXPERT, D), D_FF_EXPERT ** -0.5),
        'final_norm_g': gain((D,)),
    }


def reference(x, mem, mix_norm_g, w_in, conv_w, conv_b, conv_ln_g, conv_ln_b,
              sgu_ln_g, sgu_ln_b, sgu_w, sgu_b, pool_w, pool_b, pool_scale, w_out,
              xattn_norm_g, mem_norm_g, xattn_wq, xattn_wk, xattn_wv, xattn_wo,
              ffn_norm_g, ffn_wg, ffn_wu, ffn_wd, router_w, moe_wg, moe_wu, moe_wd,
              final_norm_g):
    a_end = 2 * D_CONV
    b_end = a_end + 2 * D_SGU
    for l in range(DEPTH):
        h = rms_norm(x, mix_norm_g[l])
        z = h @ w_in[l]
        y_a = conformer_conv(z[..., :a_end], conv_w[l], conv_b[l], conv_ln_g[l], conv_ln_b[l])
        y_b = spatial_gating(z[..., a_end:b_end], sgu_ln_g[l], sgu_ln_b[l], sgu_w[l], sgu_b[l])
        y_c = multiscale_pool(z[..., b_end:], pool_w[l], pool_b[l], pool_scale[l])
        mixed = jnp.concatenate([y_a, y_b, y_c], axis=-1)
        x = x + mixed @ w_out[l]
        h = rms_norm(x, xattn_norm_g[l])
        m = rms_norm(mem, mem_norm_g[l])
        x = x + memory_cross_attention(h, m, xattn_wq[l], xattn_wk[l], xattn_wv[l], xattn_wo[l])
        h = rms_norm(x, ffn_norm_g[l])
        if l % 2 == 0:
            j = l // 2
            x = x + swiglu(h, ffn_wg[j], ffn_wu[j], ffn_wd[j])
        else:
            j = l // 2
            x = x + moe_swiglu(h, router_w[j], moe_wg[j], moe_wu[j], moe_wd[j])
    return rms_norm(x, final_norm_g)
```

```python
import contextlib
import numpy as np
import concourse.bass as bass
import concourse.mybir as mybir
from concourse.bass_utils import run_bass_kernel_spmd

F32 = mybir.dt.float32
BF16 = mybir.dt.bfloat16
AF = mybir.ActivationFunctionType
ALU = mybir.AluOpType
AX = mybir.AxisListType

ENGS = ("pe", "act", "dve", "pool", "sp")
SAME_ENGINE_SYNC = True

D = 1024
KC = 8
D_IN = 1792
DFF = 2816
NCH = DFF // 128
NE = 8
MEM = 256
EPS = 1e-6
BLK = 512
NV = 64
NBC = 384 + 384 + 512
CAP = 384
NS = CAP // 128
I32 = mybir.dt.int32


class Buf:
    __slots__ = ("name", "writers", "readers", "psum")

    def __init__(self, name="", psum=False):
        self.name = name
        self.psum = psum
        self.writers = []
        self.readers = []


class Op:
    __slots__ = ("eng", "fn", "deps", "signals", "sigval", "dma_sem", "dma_val", "is_dma", "dma_need", "cond", "regload")

    def __init__(self, eng, fn, is_dma=False):
        self.cond = None
        self.regload = None
        self.eng = eng
        self.fn = fn
        self.deps = set()
        self.signals = False
        self.sigval = None
        self.is_dma = is_dma
        self.dma_sem = None
        self.dma_val = None
        self.dma_need = {}


class Prog:
    def __init__(self):
        self.ops = {e: [] for e in ENGS}
        self.all_ops = []
        self.dma_sem_count = {}
        self.cur_cond = None
        self.n_cond = 0

    def cond_begin(self, flag_ap, b_flag):
        assert self.cur_cond is None
        self.n_cond += 1
        cid = self.n_cond
        for e in ("pe", "act", "dve"):
            o = Op(e, None)
            o.regload = flag_ap
            self._track(o, [b_flag], [])
            b_flag.readers.remove(o)
            self.ops[e].append(o)
            self.all_ops.append(o)
        self.cur_cond = cid

    def cond_end(self):
        self.cur_cond = None

    def _add_dep(self, op, d):
        if d is op:
            return
        if d.is_dma:
            op.dma_need[d.dma_sem] = self.dma_sem_count[d.dma_sem]
        else:
            op.deps.add(d)

    def _track(self, op, reads, writes):
        for b in reads:
            for w in b.writers:
                self._add_dep(op, w)
            if b.psum:
                for r in b.readers:
                    if r.eng != op.eng:
                        self._add_dep(op, r)
            b.readers.append(op)
            if len(b.readers) > 96:
                b.readers = b.readers[-96:]
        for b in writes:
            if b.readers:
                for r in b.readers:
                    self._add_dep(op, r)
                for w in b.writers:
                    self._add_dep(op, w)
                b.writers = [op]
                b.readers = []
            else:
                for w in b.writers:
                    if not (w.is_dma and op.is_dma):
                        self._add_dep(op, w)
                b.writers.append(op)
                if len(b.writers) > 96:
                    b.writers = b.writers[-96:]

    def op(self, eng, fn, reads=(), writes=()):
        o = Op(eng, fn)
        o.cond = self.cur_cond
        self._track(o, reads, writes)
        self.ops[eng].append(o)
        self.all_ops.append(o)
        return o

    def dma(self, eng, semkey, fn, reads=(), writes=()):
        assert self.cur_cond is None
        o = Op(eng, fn, is_dma=True)
        self._track(o, reads, writes)
        v = self.dma_sem_count.get(semkey, 0) + 16
        self.dma_sem_count[semkey] = v
        o.dma_sem = semkey
        o.dma_val = v
        self.ops[eng].append(o)
        self.all_ops.append(o)
        return o

    def emit(self, nc, final_waits=()):
        def skip(d, o):
            return d.eng == o.eng and (d.eng == "pe" or not SAME_ENGINE_SYNC) and not o.is_dma

        for o in self.all_ops:
            for d in o.deps:
                if not skip(d, o):
                    d.signals = True
        for e in ENGS:
            k = 0
            for o in self.ops[e]:
                if o.signals and not o.is_dma:
                    k += 1
                    o.sigval = k
        es = contextlib.ExitStack()
        with es:
            esem = {e: es.enter_context(nc.semaphore("s_" + e)) for e in ENGS}
            dsem = {k: es.enter_context(nc.semaphore("d_%s" % (k,))) for k in self.dma_sem_count}
            block = es.enter_context(nc.Block())
            prog = self

            def emit_one(e, engine, o, waited):
                need = {}
                for sk, val in o.dma_need.items():
                    need[("d", sk)] = val
                for d in o.deps:
                    if skip(d, o):
                        continue
                    key = ("e", d.eng)
                    if d.sigval > need.get(key, 0):
                        need[key] = d.sigval
                for key, val in need.items():
                    if waited.get(key, 0) >= val:
                        continue
                    waited[key] = val
                    engine.wait_ge(dsem[key[1]] if key[0] == "d" else esem[key[1]], val)
                if o.regload is not None:
                    ins = engine.load(creg[e], o.regload)
                    if o.signals:
                        ins.then_inc(esem[e], 1)
                    return
                ins = o.fn(engine)
                if o.is_dma:
                    ins.then_inc(dsem[o.dma_sem], 16)
                elif o.signals:
                    ins.then_inc(esem[e], 1)

            creg = {}

            def run(e, engine):
                waited = {}
                if e in ("pe", "act", "dve") and prog.n_cond:
                    creg[e] = engine.alloc_register("cflag_" + e)
                lst = prog.ops[e]
                i = 0
                while i < len(lst):
                    o = lst[i]
                    if o.cond is None:
                        emit_one(e, engine, o, waited)
                        i += 1
                        continue
                    j = i
                    while j < len(lst) and lst[j].cond == o.cond:
                        j += 1
                    grp = lst[i:j]
                    nsig = sum(1 for g in grp if g.signals)
                    w2 = dict(waited)
                    with engine.If_ne(creg[e], 0):
                        for g in grp:
                            emit_one(e, engine, g, w2)
                    if nsig:
                        with engine.Else():
                            engine.drain().then_inc(esem[e], nsig)
                    i = j
                if e == "sp":
                    done = set()
                    for o in final_waits:
                        if o.dma_sem not in done:
                            done.add(o.dma_sem)
                            engine.wait_ge(dsem[o.dma_sem], prog.dma_sem_count[o.dma_sem])

            @block.tensor
            def _(eng):
                run("pe", eng)

            @block.scalar
            def _(eng):
                run("act", eng)

            @block.vector
            def _(eng):
                run("dve", eng)

            @block.gpsimd
            def _(eng):
                run("pool", eng)

            @block.sync
            def _(eng):
                run("sp", eng)


def build_program(NTOK, NBLK=2, n_layers=2, G=4, NSLOT=2):
    PT = NBLK * BLK
    assert NTOK % PT == 0
    NPASS = NTOK // PT
    nc = bass.Bass("TRN2", target_bir_lowering=False)

    def din(name, shape):
        return nc.dram_tensor(name, list(shape), F32, kind="ExternalInput").ap()

    x_d = din("x", [NTOK, D])
    mem_d = din("mem", [MEM, D])
    w_in_d = din("w_in", [2, D, D_IN])
    w_out_d = din("w_out", [2, D, D])
    wq_d = din("xattn_wq", [2, D, D])
    wk_d = din("xattn_wk", [2, D, D])
    wv_d = din("xattn_wv", [2, D, D])
    wo_d = din("xattn_wo", [2, D, D])
    fwg_d = din("ffn_wg", [1, D, DFF])
    fwu_d = din("ffn_wu", [1, D, DFF])
    fwd_d = din("ffn_wd", [1, DFF, D])
    rw_d = din("router_w", [D, NE])
    mwg_d = din("moe_wg", [NE, D, DFF])
    mwu_d = din("moe_wu", [NE, D, DFF])
    mwd_d = din("moe_wd", [NE, DFF, D])
    vecs_d = din("vecs", [2, 128, NV])
    bc_d = din("bcast", [2, 128, NBC])
    cw_d = din("convw_t", [2, 128, 3 * 31])
    sguwt_d = din("sguw_t", [2, 128, 4 * 128])
    poolw_d = din("pool_w", [2, 4, 64, 64])
    consts_d = din("consts", [128, 128 * 16])
    iota_d = din("iota", [128, CAP])
    y_d = nc.dram_tensor("y", [NTOK, D], F32, kind="ExternalOutput").ap()

    P = Prog()
    es = contextlib.ExitStack()
    with es:
        def sb(name, shape, dt):
            return es.enter_context(nc.sbuf_tensor(name, list(shape), dt))

        xT = sb("xT", [128, KC, PT], F32)
        b_xT = [[Buf("xT%d_%d" % (k, b)) for b in range(NBLK)] for k in range(KC)]
        consts = sb("consts_sb", [128, 3, 128], F32)
        b_consts = Buf("consts")
        scr = sb("scr", [128, 4], F32)
        b_scr = Buf("scr")
        identb = sb("identb", [128, 128], BF16)
        onesb = sb("onesb", [128, 128], BF16)
        ptm = sb("ptm", [128, 12, 128], BF16)
        trib = sb("trib", [128, 128], BF16)
        iota = sb("iota_sb", [128, CAP], F32)
        b_iota = Buf("iota")
        b_cb16 = Buf("constsb16")
        vecs = sb("vecs_sb", [128, 2, NV], F32)
        b_vecs = Buf("vecs")
        bct = sb("bct", [128, NBC], F32)
        b_bct = Buf("bct")
        tail = sb("tail", [128, 2, 3, 30], BF16)
        b_tail = [Buf("tail%d" % l) for l in range(2)]
        cprev = sb("cprev", [128, 2, 256], BF16)
        b_cprev = [Buf("cprev%d" % l) for l in range(2)]

        ident = consts[:, 0, :]
        maskT = consts[:, 1, :]
        onesf = consts[:, 2, :]

        class Arena:
            def __init__(self, name, cols, dt):
                self.t = sb(name, [128, cols], dt)
                self.cols = cols
                self.prev = []
                self.cur = []

            def new_phase(self):
                self.prev = self.cur
                self.cur = []

            def carve(self, off, shape, name, parts=128, nbuf=1):
                n = 1
                for s_ in shape[1:]:
                    n *= s_
                assert off + n <= self.cols, (name, off, n, self.cols)
                v = self.t[0:parts, off:off + n]
                if len(shape) == 3:
                    v = v.rearrange("p (a b) -> p a b", a=shape[1])
                elif len(shape) == 4:
                    v = v.rearrange("p (a b c) -> p a b c", a=shape[1], b=shape[2])
                bs = [Buf("%s%d" % (name, i)) for i in range(nbuf)]
                self.cur.extend(bs)
                return v, (bs[0] if nbuf == 1 else bs)

        WA = Arena("arena", 37504, BF16)
        TF = Arena("tmpf", 4608, F32)
        TB = Arena("tmpb", 23552, BF16)

        def new_phase():
            for a in (WA, TF, TB):
                a.new_phase()
            prev = WA.prev + TF.prev + TB.prev
            if prev:
                P.op("dve", lambda e: e.memset(scr[:, 0:1], 0.0), reads=[], writes=[b_scr] + prev)
                P.op("act", lambda e: e.copy(scr[:, 1:2], scr[:, 2:3]), reads=[], writes=[b_scr] + prev)
            return list(WA.prev)

        sq = sb("sq", [128, 2, BLK], BF16)
        b_sq = [Buf("sq0"), Buf("sq1")]
        t1 = sb("t1", [128, BLK], F32)
        t2 = sb("t2", [128, BLK], F32)
        t3 = sb("t3", [128, BLK], F32)
        t4 = sb("t4", [128, BLK], F32)
        b_t1, b_t2, b_t3, b_t4 = Buf("t1"), Buf("t2"), Buf("t3"), Buf("t4")
        st = sb("st", [128, 8, 4], F32)
        b_st = Buf("st")
        xin = sb("xin", [128, 2, D], F32)
        b_xin = [Buf("xin0"), Buf("xin1")]
        NT_ = PT // 128
        rl = sb("rl", [128, NT_, 8], F32)
        b_rl = Buf("rl")
        rt = sb("rt", [128, 6, NT_, 8], F32)
        b_rt = Buf("rt")
        rs = sb("rs", [128, 8, NT_], F32)
        b_rs = Buf("rs")
        rs2 = sb("rs2", [128, 2, 8], F32)
        b_rs2 = Buf("rs2")
        rankm = sb("rankm", [128, NT_, 8], F32)
        b_rankm = Buf("rankm")
        maskb = sb("maskb", [128, NT_, 8], BF16)
        b_maskb = Buf("maskb")
        combhl = sb("combhl", [128, 2, NT_, 8], BF16)
        b_combhl = Buf("combhl")
        cbs = sb("cbs", [128, NS], F32)
        b_cbs = Buf("cbs")
        flag_i = sb("flag_i", [128, 8], I32)
        b_flag = Buf("flag")
        dg = sb("dg", [128, 4, 128], F32)
        b_dg = [Buf("dg%d" % i) for i in range(4)]
        rwb = sb("rwb", [128, KC, NE], BF16)
        b_rwb = Buf("rwb")
        wmt = sb("wmt", [128, 4, 128], BF16)
        b_wmt = Buf("wmt")
        plw = sb("plw", [64, 4, 64], BF16)
        b_plw = Buf("plw")

        banks = [es.enter_context(nc.psum_tensor("pb%d" % i, [128, BLK], F32)) for i in range(8)]
        b_banks = [Buf("pb%d" % i, psum=True) for i in range(8)]
        bank_rr = [0]

        def nb():
            i = bank_rr[0]
            bank_rr[0] = (i + 1) % 8
            return banks[i], b_banks[i]

        def mm(out, lhsT, rhs, start, stop, reads, wbuf):
            P.op("pe", lambda e: e.matmul(out, lhsT, rhs, start=start, stop=stop), reads=reads, writes=[wbuf])

        def act(out, in_, func, reads, writes, bias=None, scale=None):
            kw = {}
            if bias is not None:
                kw["bias"] = bias
            if scale is not None:
                kw["scale"] = scale
            P.op("act", lambda e: e.activation(out, in_, func, **kw), reads=reads, writes=writes)

        def dve(fn, reads, writes):
            P.op("dve", fn, reads=reads, writes=writes)

        def wdma(out, in_, sem, wbuf, extra_w=()):
            return P.dma("pool", sem, lambda e: e.dma_start(out=out, in_=in_), writes=[wbuf] + list(extra_w))

        def sdma(out, in_, sem, reads=(), writes=()):
            return P.dma("sp", sem, lambda e: e.dma_start(out=out, in_=in_), reads=reads, writes=writes)

        sdma(consts[:], consts_d[:, 0:384].rearrange("p (a b) -> p a b", a=3), "c0", writes=[b_consts])
        sdma(vecs[:], vecs_d.rearrange("l p n -> p l n"), "c1", writes=[b_vecs])
        wdma(identb[:], consts_d[:, 0:128], "c2", b_cb16)
        wdma(onesb[:], consts_d[:, 256:384], "c2", b_cb16)
        wdma(ptm[:], consts_d[:, 384:1920].rearrange("p (a b) -> p a b", a=12), "c2", b_cb16)
        wdma(trib[:], consts_d[:, 1920:2048], "c2", b_cb16)
        sdma(iota[:], iota_d, "c0", writes=[b_iota])
        dve(lambda e: e.memset(scr[:], 0.0), [], [b_scr])
        wdma(rwb[:], rw_d.rearrange("(k p) e -> p k e", p=128), "c2", b_rwb)

        def vcol(l, c, parts=128):
            return vecs[0:parts, l, c:c + 1]

        def rmsnorm(src, b_src, N, gl, gcol, out, b_out_list, out_f32=False):
            bk, bb = nb()
            for kc in range(KC):
                s = kc % 2
                act(sq[:, s, 0:N], src(kc), AF.Square, [b_src[kc]], [b_sq[s]])
                mm(bk[:, 0:N], onesb[:], sq[:, s, 0:N], kc == 0, kc == KC - 1, [b_sq[s], b_cb16], bb)
            act(t1[:, 0:N], bk[:, 0:N], AF.Sqrt, [bb], [b_t1], bias=EPS, scale=1.0 / D)
            dve(lambda e: e.reciprocal(t2[:, 0:N], t1[:, 0:N]), [b_t1], [b_t2])
            for kc in range(KC):
                o_ap, s_ap, g_ap = out(kc), src(kc), vcol(gl, gcol + kc)
                dve(lambda e, o_ap=o_ap, s_ap=s_ap, g_ap=g_ap: e.scalar_tensor_tensor(o_ap, s_ap, g_ap, t2[:, 0:N],
                                                                                     ALU.mult, ALU.mult),
                    [b_src[kc], b_t2, b_vecs], [b_out_list[kc]])

        def add_resid(dc, b, bk, bb):
            sl = slice(b * BLK, (b + 1) * BLK)
            dve(lambda e: e.tensor_tensor(xT[:, dc, sl], bk[:], xT[:, dc, sl], ALU.add), [bb, b_xT[dc][b]], [b_xT[dc][b]])

        out_dmas = []
        for ps_i in range(NPASS):
            tok0 = ps_i * PT
            for tt in range(PT // 128):
                s = tt % 2
                b = tt // 4
                sdma(xin[:, s, :], x_d[tok0 + tt * 128: tok0 + (tt + 1) * 128, :], "xin%d" % s, writes=[b_xin[s]])
                for half in range(2):
                    bk, bb = nb()
                    for j in range(4):
                        kc = half * 4 + j
                        P.op("pe", lambda e, kc=kc, j=j, bk=bk, s=s: e.transpose(bk[:, j * 128:(j + 1) * 128],
                                                                               xin[:, s, kc * 128:(kc + 1) * 128], ident),
                             reads=[b_xin[s], b_consts], writes=[bb])
                    P.op("act", lambda e, half=half, bk=bk, tt=tt: e.copy(
                        xT[:, half * 4:(half + 1) * 4, tt * 128:(tt + 1) * 128],
                        bk[:].rearrange("p (a b) -> p a b", a=4)),
                        reads=[bb], writes=[b_xT[half * 4 + j][b] for j in range(4)])

            for l in range(n_layers):
                ov = new_phase()
                win, b_win = WA.carve(0, [128, KC, D_IN], "win")
                woA, b_woA = WA.carve(14336, [128, 3, D], "woA")
                woB, b_woB = WA.carve(14336 + 3072, [96, 4, D], "woB", parts=96)
                woC, b_woC = WA.carve(14336 + 3072 + 4096, [64, 4, D], "woC", parts=64)
                Dm, b_Dm = WA.carve(14336 + 3072 + 8192, [128, 93, 128], "Dm")
                hc, b_hc = TF.carve(0, [128, 3, BLK], "hc", nbuf=3)
                gv, b_gv = TF.carve(1536, [128, 4, 384], "gv", nbuf=4)
                gsq, b_gsq = TF.carve(3072, [128, 4, 384], "gsq")
                glu, b_glu = TB.carve(0, [128, 3, 30 + BLK], "glu", nbuf=3)
                ya, b_ya = TB.carve(1626, [128, 3, BLK], "ya", nbuf=3)
                ug, b_ug = TB.carve(3162, [128, 4, BLK], "ug", nbuf=4)
                vn, b_vn = TB.carve(5210, [128, 4, 384], "vn", nbuf=4)
                cT, b_cT = TB.carve(6746, [128, 5, 256], "cT", nbuf=5)
                yb, b_yb = TB.carve(8026, [128, 4, BLK], "yb")
                pTm, b_pTm = TB.carve(10074, [128, 4, BLK], "pTm")
                yc, b_yc = TB.carve(12122, [128, 4, BLK], "yc")
                hTb0, b_hTb0 = TB.carve(14200, [128, KC, BLK], "hTb0")
                hTb1, b_hTb1 = TB.carve(18296, [128, KC, BLK], "hTb1")
                hTbs = [(hTb0, b_hTb0), (hTb1, b_hTb1)]
                for kh in range(2):
                    wdma(win[:, kh * 4:(kh + 1) * 4, :],
                         w_in_d[l, kh * 512:(kh + 1) * 512, :].rearrange("(k p) n -> p k n", p=128), "wA", b_win, ov)
                wdma(woA, w_out_d[l, 0:384, :].rearrange("(j p) n -> p j n", p=128), "wA", b_woA, ov)
                wdma(woB, w_out_d[l, 384:768, :].rearrange("(j p) n -> p j n", p=96), "wA", b_woB, ov)
                wdma(woC, w_out_d[l, 768:1024, :].rearrange("(j p) n -> p j n", p=64), "wA", b_woC, ov)
                sdma(t3[:, 0:93], cw_d[l], "c3", writes=[b_t3])
                for idx in range(93):
                    dve(lambda e, idx=idx: e.tensor_scalar(Dm[:, idx, :], identb[:], t3[:, idx:idx + 1], None, ALU.mult),
                        [b_t3, b_cb16], [b_Dm])
                sdma(bct[:], bc_d[l], "c4", writes=[b_bct])
                sdma(t4[:, 0:512], sguwt_d[l], "c5", writes=[b_t4])
                for h in range(4):
                    dve(lambda e, h=h: e.tensor_tensor(wmt[:, h, :], t4[:, h * 128:(h + 1) * 128], maskT, ALU.mult),
                        [b_t4, b_consts], [b_wmt])
                wdma(plw[:], poolw_d[l].rearrange("g i o -> i g o"), "c6", b_plw)

                for b in range(NBLK):
                    gb = ps_i * NBLK + b
                    sl = slice(b * BLK, (b + 1) * BLK)

                    def mix_norm(bb_):
                        hb, bhb = hTbs[bb_ % 2]
                        sl_ = slice(bb_ * BLK, (bb_ + 1) * BLK)
                        rmsnorm(lambda kc: xT[:, kc, sl_], [b_xT[kc][bb_] for kc in range(KC)], BLK, l, 0,
                                lambda kc: hb[:, kc, :], [bhb] * KC)
                    if b == 0:
                        mix_norm(0)
                    hTb, b_hTb = hTbs[b % 2]
                    if gb == 0:
                        dve(lambda e: e.memset(glu[:, :, 0:30], 0.0), [], b_glu)
                    else:
                        dve(lambda e, l=l: e.tensor_copy(glu[:, :, 0:30], tail[:, l, :, :]), [b_tail[l]], b_glu)
                    for j in range(3):
                        bv, bbv = nb()
                        bg, bbg = nb()
                        for kc in range(KC):
                            mm(bv[:], win[:, kc, j * 128:(j + 1) * 128], hTb[:, kc, :], kc == 0, kc == KC - 1, [b_win, b_hTb], bbv)
                        for kc in range(KC):
                            mm(bg[:], win[:, kc, (3 + j) * 128:(4 + j) * 128], hTb[:, kc, :], kc == 0, kc == KC - 1, [b_win, b_hTb], bbg)
                        act(t3[:], bg[:], AF.Sigmoid, [bbg], [b_t3])
                        dve(lambda e, j=j, bv=bv: e.tensor_tensor(glu[:, j, 30:30 + BLK], bv[:], t3[:], ALU.mult),
                            [bbv, b_t3], [b_glu[j]])
                    dve(lambda e, l=l: e.tensor_copy(tail[:, l, :, :], glu[:, :, BLK:BLK + 30]), b_glu, [b_tail[l]])
                    bs1, bbs1 = nb()
                    bs2, bbs2 = nb()
                    for j in range(3):
                        bc_, bbc = nb()
                        for tap in range(31):
                            mm(bc_[:], Dm[:, j * 31 + tap, :], glu[:, j, tap:tap + BLK], tap == 0, tap == 30, [b_Dm, b_glu[j]], bbc)
                        act(hc[:, j, :], bc_[:], AF.Identity, [bbc, b_vecs], [b_hc[j]], bias=vcol(l, 32 + j))
                        s = j % 2
                        act(sq[:, s, :], hc[:, j, :], AF.Square, [b_hc[j]], [b_sq[s]])
                        mm(bs2[:], onesb[:], sq[:, s, :], j == 0, j == 2, [b_sq[s], b_cb16], bbs2)
                    for j in range(3):
                        dve(lambda e, j=j: e.tensor_copy(ya[:, j, :], hc[:, j, :]), [b_hc[j]], [b_ya[j]])
                        mm(bs1[:], onesb[:], ya[:, j, :], j == 0, j == 2, [b_ya[j], b_cb16], bbs1)
                    act(t1[:], bs1[:], AF.Identity, [bbs1], [b_t1], scale=1.0 / 384)
                    dve(lambda e: e.tensor_tensor(t2[:], t1[:], t1[:], ALU.mult), [b_t1], [b_t2])
                    dve(lambda e, bs2=bs2: e.scalar_tensor_tensor(t3[:], bs2[:], 1.0 / 384, t2[:], ALU.mult, ALU.subtract),
                        [bbs2, b_t2], [b_t3])
                    act(t2[:], t3[:], AF.Sqrt, [b_t3], [b_t2], bias=EPS, scale=1.0)
                    dve(lambda e: e.reciprocal(t3[:], t2[:]), [b_t2], [b_t3])
                    for h in range(4):
                        bu, bbu = nb()
                        for kc in range(KC):
                            mm(bu[0:96, :], win[:, kc, 768 + 96 * h:768 + 96 * (h + 1)], hTb[:, kc, :], kc == 0, kc == KC - 1,
                               [b_win, b_hTb], bbu)
                        act(ug[0:96, h, :], bu[0:96, :], AF.Gelu_apprx_tanh, [bbu], [b_ug[h]])
                    if gb == 0:
                        pass
                    else:
                        dve(lambda e, l=l: e.tensor_copy(cT[:, 0, :], cprev[:, l, :]), [b_cprev[l]], [b_cT[0]])
                    for tt in range(4):
                        ba, bba = nb()
                        bq, bbq = nb()
                        tsl = slice(tt * 128, (tt + 1) * 128)
                        for kc in range(KC):
                            mm(ba[:, 0:320], hTb[:, kc, tsl], win[:, kc, 1152:1472], kc == 0, kc == KC - 1, [b_win, b_hTb], bba)
                        for kc in range(KC):
                            mm(bq[:, 0:320], hTb[:, kc, tsl], win[:, kc, 1472:1792], kc == 0, kc == KC - 1, [b_win, b_hTb], bbq)
                        act(gv[:, tt, 0:320], ba[:, 0:320], AF.Gelu_apprx_tanh, [bba], [b_gv[tt]])
                        act(gv[:, tt, 320:384], bq[:, 0:64], AF.Gelu_apprx_tanh, [bbq], [b_gv[tt]])
                        dve(lambda e, tt=tt, bq=bq: e.tensor_copy(cT[:, 1 + tt, :], bq[:, 64:320]), [bbq], [b_cT[1 + tt]])
                    dve(lambda e, l=l: e.tensor_copy(cprev[:, l, :], cT[:, 4, :]), [b_cT[4]], [b_cprev[l]])
                    dve(lambda e: e.tensor_reduce(st[:, 0, :], gv[:], AX.X, ALU.add), b_gv, [b_st])
                    dve(lambda e: e.tensor_tensor(gsq[:], gv[:], gv[:], ALU.mult), b_gv, [b_gsq])
                    dve(lambda e: e.tensor_reduce(st[:, 1, :], gsq[:], AX.X, ALU.add), [b_gsq], [b_st])
                    dve(lambda e: e.tensor_scalar(st[:, 2, :], st[:, 0, :], 1.0 / 384, None, ALU.mult), [b_st], [b_st])
                    dve(lambda e: e.tensor_tensor(st[:, 3, :], st[:, 2, :], st[:, 2, :], ALU.mult), [b_st], [b_st])
                    dve(lambda e: e.scalar_tensor_tensor(st[:, 4, :], st[:, 1, :], 1.0 / 384, st[:, 3, :], ALU.mult, ALU.subtract),
                        [b_st], [b_st])
                    act(st[:, 5, :], st[:, 4, :], AF.Sqrt, [b_st], [b_st], bias=EPS, scale=1.0)
                    dve(lambda e: e.reciprocal(st[:, 6, :], st[:, 5, :]), [b_st], [b_st])
                    for tt in range(4):
                        dve(lambda e, tt=tt: e.tensor_scalar(gsq[:, tt, :], gv[:, tt, :], st[:, 2, tt:tt + 1], st[:, 6, tt:tt + 1],
                                                             ALU.subtract, ALU.mult), [b_gv[tt], b_st], [b_gsq])
                        dve(lambda e, tt=tt: e.tensor_tensor(gsq[:, tt, :], gsq[:, tt, :], bct[:, 0:384], ALU.mult), [b_gsq, b_bct], [b_gsq])
                        dve(lambda e, tt=tt: e.tensor_tensor(vn[:, tt, :], gsq[:, tt, :], bct[:, 384:768], ALU.add), [b_gsq, b_bct], [b_vn[tt]])
                    for tt in range(4):
                        tsl = slice(tt * 128, (tt + 1) * 128)
                        bS, bbS = nb()
                        for h in range(4):
                            mm(bS[0:96, h * 128:(h + 1) * 128], vn[:, tt, 96 * h:96 * (h + 1)], wmt[:, h, :], True, True,
                               [b_vn[tt], b_wmt], bbS)
                        dve(lambda e, bS=bS: e.tensor_tensor(t4[0:96, :], bS[0:96, :], bct[0:96, 768:1280], ALU.add), [bbS, b_bct], [b_t4])
                        dve(lambda e, tsl=tsl: e.tensor_tensor(yb[0:96, :, tsl], t4[0:96, :].rearrange("p (a b) -> p a b", a=4),
                                                               ug[0:96, :, tsl], ALU.mult), [b_t4] + b_ug, [b_yb])
                        bP, bbP = nb()
                        first = (gb == 0 and tt == 0)
                        for g in range(4):
                            if first:
                                mm(bP[0:64, g * 128:(g + 1) * 128], cT[:, 1 + tt, 64 * g:64 * (g + 1)], ptm[:, 8 + g, :], True, True,
                                   [b_cT[1 + tt], b_cb16], bbP)
                            else:
                                mm(bP[0:64, g * 128:(g + 1) * 128], cT[:, 1 + tt, 64 * g:64 * (g + 1)], ptm[:, g, :], True, False,
                                   [b_cT[1 + tt], b_cb16], bbP)
                                mm(bP[0:64, g * 128:(g + 1) * 128], cT[:, tt, 64 * g:64 * (g + 1)], ptm[:, 4 + g, :], False, True,
                                   [b_cT[tt], b_cb16], bbP)
                        P.op("act", lambda e, bP=bP, tsl=tsl: e.copy(pTm[0:64, :, tsl], bP[0:64, :].rearrange("p (a b) -> p a b", a=4)),
                             reads=[bbP], writes=[b_pTm])
                    for g in range(4):
                        bQ, bbQ = nb()
                        mm(bQ[0:64, :], plw[:, g, :], pTm[0:64, g, :], True, True, [b_plw, b_pTm], bbQ)
                        dve(lambda e, g=g, bQ=bQ, l=l: e.tensor_scalar(yc[0:64, g, :], bQ[0:64, :], vcol(l, 41 + g, 64), vcol(l, 45 + g, 64),
                                                                   ALU.add, ALU.mult), [bbQ, b_vecs], [b_yc])
                    for j in range(3):
                        dve(lambda e, j=j: e.tensor_tensor(t4[:], hc[:, j, :], t1[:], ALU.subtract), [b_hc[j], b_t1], [b_t4])
                        dve(lambda e, j=j: e.tensor_tensor(t4[:], t4[:], t3[:], ALU.mult), [b_t4, b_t3], [b_t4])
                        act(ya[:, j, :], t4[:], AF.Silu, [b_t4, b_vecs], [b_ya[j]], bias=vcol(l, 38 + j), scale=vcol(l, 35 + j))
                    if b + 1 < NBLK:
                        mix_norm(b + 1)
                    for dc in range(KC):
                        bo, bbo = nb()
                        dsl = slice(dc * 128, (dc + 1) * 128)
                        for j in range(3):
                            mm(bo[:], woA[:, j, dsl], ya[:, j, :], j == 0, False, [b_woA, b_ya[j]], bbo)
                        for h in range(4):
                            mm(bo[:], woB[:, h, dsl], yb[0:96, h, :], False, False, [b_woB, b_yb], bbo)
                        for g in range(4):
                            mm(bo[:], woC[:, g, dsl], yc[0:64, g, :], False, g == 3, [b_woC, b_yc], bbo)
                        add_resid(dc, b, bo, bbo)

                ov = new_phase()
                wq, b_wq = WA.carve(0, [128, KC, D], "wq")
                wo, b_wo = WA.carve(8192, [128, KC, D], "wo")
                wkv, b_wkv = WA.carve(16384, [128, KC, D], "wkv")
                kT, b_kT = WA.carve(24576, [128, KC, MEM], "kT")
                Vt, b_Vt = WA.carve(26624, [128, 2, D], "Vt")
                mT, b_mT = WA.carve(28672, [128, KC, MEM], "mT")
                memT, b_memT = TF.carve(0, [128, KC, MEM], "memT")
                qT, b_qT = TB.carve(0, [128, KC, BLK], "qT", nbuf=KC)
                aT, b_aT = TB.carve(4096, [128, KC, BLK], "aT", nbuf=KC)
                pex, b_pex = TB.carve(8192, [128, 2, BLK], "pex", nbuf=2)
                hTb0, b_hTb0 = TB.carve(9216, [128, KC, BLK], "hTb0")
                hTb1, b_hTb1 = TB.carve(13312, [128, KC, BLK], "hTb1")
                hTbs = [(hTb0, b_hTb0), (hTb1, b_hTb1)]
                wdma(wkv, wk_d[l].rearrange("(k p) n -> p k n", p=128), "wB", b_wkv, ov)
                wdma(wq, wq_d[l].rearrange("(k p) n -> p k n", p=128), "wB", b_wq, ov)
                wdma(wo, wo_d[l].rearrange("(k p) n -> p k n", p=128), "wB", b_wo, ov)
                for mt in range(2):
                    sdma(xin[:, mt, :], mem_d[mt * 128:(mt + 1) * 128, :], "xin%d" % mt, writes=[b_xin[mt]])
                    for half in range(2):
                        bk, bb = nb()
                        for j in range(4):
                            kc = half * 4 + j
                            P.op("pe", lambda e, kc=kc, j=j, bk=bk, mt=mt: e.transpose(bk[:, j * 128:(j + 1) * 128],
                                                                                     xin[:, mt, kc * 128:(kc + 1) * 128], ident),
                                 reads=[b_xin[mt], b_consts], writes=[bb])
                        P.op("act", lambda e, half=half, bk=bk, mt=mt: e.copy(
                            memT[:, half * 4:(half + 1) * 4, mt * 128:(mt + 1) * 128],
                            bk[:].rearrange("p (a b) -> p a b", a=4)), reads=[bb], writes=[b_memT])
                rmsnorm(lambda kc: memT[:, kc, :], [b_memT] * KC, MEM, l, 16, lambda kc: mT[:, kc, :], [b_mT] * KC)
                for dc in range(KC):
                    bk, bb = nb()
                    for kc in range(KC):
                        mm(bk[:, 0:MEM], wkv[:, kc, dc * 128:(dc + 1) * 128], mT[:, kc, :], kc == 0, kc == KC - 1, [b_wkv, b_mT], bb)
                    P.op("act", lambda e, dc=dc, bk=bk: e.copy(kT[:, dc, :], bk[:, 0:MEM]), reads=[bb], writes=[b_kT])
                wdma(wkv, wv_d[l].rearrange("(k p) n -> p k n", p=128), "wB", b_wkv)
                for mt in range(2):
                    for hh in range(2):
                        bk, bb = nb()
                        for kc in range(KC):
                            mm(bk[:], mT[:, kc, mt * 128:(mt + 1) * 128], wkv[:, kc, hh * 512:(hh + 1) * 512], kc == 0, kc == KC - 1,
                               [b_wkv, b_mT], bb)
                        P.op("act", lambda e, mt=mt, hh=hh, bk=bk: e.copy(Vt[:, mt, hh * 512:(hh + 1) * 512], bk[:]), reads=[bb], writes=[b_Vt])
                for b in range(NBLK):
                    sl = slice(b * BLK, (b + 1) * BLK)

                    def att_norm(bb_):
                        hb, bhb = hTbs[bb_ % 2]
                        sl_ = slice(bb_ * BLK, (bb_ + 1) * BLK)
                        rmsnorm(lambda kc: xT[:, kc, sl_], [b_xT[kc][bb_] for kc in range(KC)], BLK, l, 8,
                                lambda kc: hb[:, kc, :], [bhb] * KC)
                    if b == 0:
                        att_norm(0)
                    hTb, b_hTb = hTbs[b % 2]
                    for dc in range(KC):
                        bk, bb = nb()
                        for kc in range(KC):
                            mm(bk[:], wq[:, kc, dc * 128:(dc + 1) * 128], hTb[:, kc, :], kc == 0, kc == KC - 1, [b_wq, b_hTb], bb)
                        P.op("act", lambda e, dc=dc, bk=bk: e.copy(qT[:, dc, :], bk[:]), reads=[bb], writes=[b_qT[dc]])
                    for h in range(4):
                        for mt in range(2):
                            bk, bb = nb()
                            for c in range(2):
                                mm(bk[:], kT[:, 2 * h + c, mt * 128:(mt + 1) * 128], qT[:, 2 * h + c, :], c == 0, c == 1,
                                   [b_kT, b_qT[2 * h + c]], bb)
                            act(pex[:, mt, :], bk[:], AF.Exp, [bb], [b_pex[mt]], scale=1.0 / 16.0)
                        bd, bbd = nb()
                        for mt in range(2):
                            mm(bd[:], onesb[:], pex[:, mt, :], mt == 0, mt == 1, [b_pex[mt], b_cb16], bbd)
                        dve(lambda e, bd=bd: e.reciprocal(t1[:], bd[:]), [bbd], [b_t1])
                        for c in range(2):
                            bo, bbo = nb()
                            for mt in range(2):
                                mm(bo[:], Vt[:, mt, (2 * h + c) * 128:(2 * h + c + 1) * 128], pex[:, mt, :], mt == 0, mt == 1,
                                   [b_Vt, b_pex[mt]], bbo)
                            dve(lambda e, h=h, c=c, bo=bo: e.tensor_tensor(aT[:, 2 * h + c, :], bo[:], t1[:], ALU.mult),
                                [bbo, b_t1], [b_aT[2 * h + c]])
                    if b + 1 < NBLK:
                        att_norm(b + 1)
                    for dc in range(KC):
                        bo, bbo = nb()
                        for kc in range(KC):
                            mm(bo[:], wo[:, kc, dc * 128:(dc + 1) * 128], aT[:, kc, :], kc == 0, kc == KC - 1, [b_wo, b_aT[kc]], bbo)
                        add_resid(dc, b, bo, bbo)

                ov = new_phase()
                moe = (l % 2 == 1)
                NT = PT // 128
                hT, _bh = WA.carve(0, [128, KC, PT], "hT")
                b_hT = [[Buf("hT%d_%d" % (k, b)) for b in range(NBLK)] for k in range(KC)]
                for k in range(KC):
                    WA.cur.extend(b_hT[k])
                wslot = []
                SLOTC = 3 * KC * G * 128
                for s_ in range(NSLOT):
                    base = KC * PT + s_ * SLOTC
                    wg_s, b_wg = WA.carve(base, [128, KC, G * 128], "wg%d" % s_)
                    wu_s, b_wu = WA.carve(base + KC * G * 128, [128, KC, G * 128], "wu%d" % s_)
                    wd_s, b_wd = WA.carve(base + 2 * KC * G * 128, [128, G, D], "wd%d" % s_)
                    wslot.append((wg_s, b_wg, wu_s, b_wu, wd_s, b_wd))
                assert NSLOT * SLOTC == 24576
                if not moe:
                    hdn, b_hdn = TB.carve(0, [128, 2, G, BLK], "hdn", nbuf=2)
                else:
                    hdn, b_hdn1 = TB.carve(0, [128, 1, G, BLK], "hdn")
                    b_hdn = [b_hdn1]
                    hTe, b_hTe = WA.carve(KC * PT + 24576, [128, KC, CAP], "hTe", nbuf=KC)
                    cbt1, b_cbt1 = TB.carve(2048, [128, PT], "cbt1")
                    hTM, b_hTM = TB.carve(3072, [128, NT, D], "hTM", nbuf=NT)
                    Sel, b_Sel = TB.carve(3072 + 8192, [128, NT, CAP], "Sel", nbuf=NT)
                    SelT, b_SelT = TB.carve(3072 + 8192 + 3072, [128, NS, PT], "SelT", nbuf=NS)
                    hid, b_hid = TB.carve(3072 + 8192 + 6144, [128, 2, G, CAP], "hid", nbuf=2)
                    Ybf, b_Ybf = TB.carve(3072 + 8192 + 9216, [128, NS, D], "Ybf", nbuf=NS)
                    Yacc, b_Yacc = TF.carve(0, [128, NS, D], "Yacc", nbuf=NS)
                NEXP = NE if moe else 1
                groups = []
                c0 = 0
                while c0 < NCH:
                    gsz = min(G, NCH - c0)
                    groups.append((c0, gsz))
                    c0 += gsz
                wunits = [(e, g0, gs) for e in range(NEXP) for (g0, gs) in groups]

                def load_w(ui):
                    e, g0, gs = wunits[ui]
                    wg_s, b_wg, wu_s, b_wu, wd_s, b_wd = wslot[ui % NSLOT]
                    if moe:
                        sg, su, sd_ = mwg_d[e], mwu_d[e], mwd_d[e]
                    else:
                        sg, su, sd_ = fwg_d[0], fwu_d[0], fwd_d[0]
                    o = ov if ui < NSLOT else []
                    cs = slice(g0 * 128, (g0 + gs) * 128)
                    wdma(wg_s[:, :, 0:gs * 128], sg[:, cs].rearrange("(k p) n -> p k n", p=128), "wF%d" % (ui % NSLOT), b_wg, o)
                    wdma(wu_s[:, :, 0:gs * 128], su[:, cs].rearrange("(k p) n -> p k n", p=128), "wF%d" % (ui % NSLOT), b_wu, o)
                    wdma(wd_s[:, 0:gs, :], sd_[cs, :].rearrange("(c p) n -> p c n", p=128), "wF%d" % (ui % NSLOT), b_wd, o)

                for b in range(NBLK):
                    sl = slice(b * BLK, (b + 1) * BLK)
                    rmsnorm(lambda kc: xT[:, kc, sl], [b_xT[kc][b] for kc in range(KC)], BLK, l, 24,
                            lambda kc: hT[:, kc, sl], [b_hT[kc][b] for kc in range(KC)])
                for ui0 in range(min(NSLOT, len(wunits))):
                    load_w(ui0)

                def gate_up(ui, b, hs, use_cb):
                    e_, g0, gs = wunits[ui]
                    wg_s, b_wg, wu_s, b_wu, wd_s, b_wd = wslot[ui % NSLOT]
                    sl = slice(b * BLK, (b + 1) * BLK)
                    for c in range(gs):
                        bg, bbg = nb()
                        bu, bbu = nb()
                        for kc in range(KC):
                            mm(bg[:], wg_s[:, kc, c * 128:(c + 1) * 128], hT[:, kc, sl], kc == 0, kc == KC - 1, [b_wg, b_hT[kc][b]], bbg)
                        for kc in range(KC):
                            mm(bu[:], wu_s[:, kc, c * 128:(c + 1) * 128], hT[:, kc, sl], kc == 0, kc == KC - 1, [b_wu, b_hT[kc][b]], bbu)
                        act(t3[:], bg[:], AF.Silu, [bbg], [b_t3])
                        if use_cb:
                            dve(lambda e, bu=bu: e.tensor_tensor(t4[:], bu[:], t3[:], ALU.mult), [bbu, b_t3], [b_t4])
                            dve(lambda e, c=c, hs=hs, sl=sl, hdn=hdn: e.tensor_tensor(hdn[:, hs, c, :], t4[:], cbt1[:, sl], ALU.mult),
                                [b_t4, b_cbt1], [b_hdn[hs]])
                        else:
                            dve(lambda e, c=c, hs=hs, bu=bu, hdn=hdn: e.tensor_tensor(hdn[:, hs, c, :], bu[:], t3[:], ALU.mult),
                                [bbu, b_t3], [b_hdn[hs]])

                def down(ui, b, hs):
                    e_, g0, gs = wunits[ui]
                    wg_s, b_wg, wu_s, b_wu, wd_s, b_wd = wslot[ui % NSLOT]
                    for dc in range(KC):
                        bo, bbo = nb()
                        for c in range(gs):
                            mm(bo[:], wd_s[:, c, dc * 128:(dc + 1) * 128], hdn[:, hs, c, :], c == 0, c == gs - 1, [b_wd, b_hdn[hs]], bbo)
                        add_resid(dc, b, bo, bbo)

                if not moe:
                    units = [(ui, b) for ui in range(len(wunits)) for b in range(NBLK)]
                    for n in range(len(units)):
                        gate_up(units[n][0], units[n][1], n % 2, False)
                        if n >= 1:
                            down(units[n - 1][0], units[n - 1][1], (n - 1) % 2)
                            ui_prev, b_prev = units[n - 1]
                            if b_prev == NBLK - 1 and ui_prev + NSLOT < len(wunits):
                                load_w(ui_prev + NSLOT)
                    down(units[-1][0], units[-1][1], (len(units) - 1) % 2)
                else:
                    bR, bbR = nb()
                    for t in range(NT):
                        for kc in range(KC):
                            mm(bR[:, t * 8:(t + 1) * 8], hT[:, kc, t * 128:(t + 1) * 128], rwb[:, kc, :], kc == 0, kc == KC - 1,
                               [b_hT[kc][t // 4], b_rwb], bbR)
                    P.op("act", lambda e, bR=bR: e.copy(rl[:], bR[:, 0:NT * 8].rearrange("p (a b) -> p a b", a=NT)), reads=[bbR], writes=[b_rl])
                    for t in range(NT):
                        L = rl[:, t, :]
                        def R(i, t=t):
                            return rt[:, i, t, :]
                        def S(i, t=t):
                            return rs[:, i, t:t + 1]
                        dve(lambda e, L=L, S=S: e.tensor_reduce(S(0), L, AX.X, ALU.max), [b_rl], [b_rs])
                        dve(lambda e, L=L, S=S, R=R: e.tensor_scalar(R(0), L, S(0), -1e30, ALU.is_equal, ALU.mult), [b_rl, b_rs], [b_rt])
                        dve(lambda e, L=L, R=R: e.tensor_tensor(R(1), R(0), L, ALU.add), [b_rl, b_rt], [b_rt])
                        dve(lambda e, S=S, R=R: e.tensor_reduce(S(1), R(1), AX.X, ALU.max), [b_rt], [b_rs])
                        dve(lambda e, L=L, S=S, R=R: e.tensor_scalar(R(2), L, S(1), None, ALU.is_ge), [b_rl, b_rs], [b_rt])
                        dve(lambda e, S=S: e.tensor_scalar(S(2), S(0), -1.0, None, ALU.mult), [b_rs], [b_rs])
                        act(R(3), L, AF.Exp, [b_rl, b_rs], [b_rt], bias=S(2))
                        dve(lambda e, R=R: e.tensor_tensor(R(4), R(3), R(2), ALU.mult), [b_rt], [b_rt])
                        dve(lambda e, S=S, R=R: e.tensor_reduce(S(3), R(4), AX.X, ALU.add), [b_rt], [b_rs])
                        dve(lambda e, S=S: e.reciprocal(S(4), S(3)), [b_rs], [b_rs])
                        dve(lambda e, S=S, R=R: e.tensor_scalar(R(5), R(4), S(4), None, ALU.mult), [b_rt, b_rs], [b_rt])
                    dve(lambda e: e.tensor_copy(maskb[:], rt[:, 2, :, :]), [b_rt], [b_maskb])
                    dve(lambda e: e.tensor_copy(combhl[:, 0, :, :], rt[:, 5, :, :]), [b_rt], [b_combhl])
                    dve(lambda e: e.tensor_tensor(rt[:, 3, :, :], rt[:, 5, :, :], combhl[:, 0, :, :], ALU.subtract), [b_rt, b_combhl], [b_rt])
                    dve(lambda e: e.tensor_copy(combhl[:, 1, :, :], rt[:, 3, :, :]), [b_rt], [b_combhl])
                    bK, bbK = nb()
                    for i in range(NT):
                        for i2 in range(i + 1):
                            mm(bK[:, i * 8:(i + 1) * 8], (onesb[:] if i2 < i else trib[:]), maskb[:, i2, :], i2 == 0, i2 == i,
                               [b_maskb, b_cb16], bbK)
                    bN, bbN = nb()
                    for i2 in range(NT):
                        mm(bN[:, 0:8], onesb[:], maskb[:, i2, :], i2 == 0, i2 == NT - 1, [b_maskb, b_cb16], bbN)
                    dve(lambda e, bN=bN: e.tensor_scalar(rs2[:, 0, :], bN[:, 0:8], float(CAP), None, ALU.is_gt), [bbN], [b_rs2])
                    dve(lambda e, bN=bN: e.tensor_scalar(rs2[:, 1, :], bN[:, 0:8], float(CAP), None, ALU.is_le), [bbN], [b_rs2])
                    dve(lambda e: e.tensor_copy(flag_i[:], rs2[:, 0, :]), [b_rs2], [b_flag])
                    for t in range(NT):
                        dve(lambda e, t=t, bK=bK: e.scalar_tensor_tensor(rankm[:, t, :], bK[:, t * 8:(t + 1) * 8], 1.0, rt[:, 2, t, :],
                                                                         ALU.add, ALU.mult), [bbK, b_rt], [b_rankm])
                        dve(lambda e, t=t: e.tensor_tensor(rankm[:, t, :], rankm[:, t, :], rs2[:, 1, :], ALU.mult), [b_rankm, b_rs2], [b_rankm])
                        dve(lambda e, t=t: e.tensor_scalar(rankm[:, t, :], rankm[:, t, :], -1.0, None, ALU.add), [b_rankm], [b_rankm])
                    for t in range(NT):
                        bk, bb = nb()
                        bkb = bk[:].bitcast(BF16)
                        for kc in range(KC):
                            P.op("pe", lambda e, kc=kc, t=t, bkb=bkb: e.transpose(bkb[:, kc * 128:(kc + 1) * 128],
                                                                                 hT[:, kc, t * 128:(t + 1) * 128], identb[:]),
                                 reads=[b_hT[kc][t // 4], b_cb16], writes=[bb])
                        P.op("act", lambda e, t=t, bkb=bkb: e.copy(hTM[:, t, :], bkb), reads=[bb], writes=[b_hTM[t]])

                    for e_ in range(NE):
                        for t in range(NT):
                            dve(lambda e, t=t, e_=e_: e.tensor_scalar(Sel[:, t, :], iota[:], rankm[:, t, e_:e_ + 1], None, ALU.is_equal),
                                [b_rankm, b_iota], [b_Sel[t]])
                        for s in range(NS):
                            bk, bb = nb()
                            bkb = bk[:].bitcast(BF16)
                            for t in range(NT):
                                P.op("pe", lambda e, t=t, s=s, bkb=bkb: e.transpose(bkb[:, t * 128:(t + 1) * 128],
                                                                                   Sel[:, t, s * 128:(s + 1) * 128], identb[:]),
                                     reads=[b_Sel[t], b_cb16], writes=[bb])
                            P.op("act", lambda e, s=s, bkb=bkb: e.copy(SelT[:, s, :], bkb), reads=[bb], writes=[b_SelT[s]])
                        bC, bbC = nb()
                        for s in range(NS):
                            n_acc = 2 * NT
                            k_acc = 0
                            for t in range(NT):
                                for hl in range(2):
                                    mm(bC[:, s:s + 1], Sel[:, t, s * 128:(s + 1) * 128], combhl[:, hl, t, e_:e_ + 1], k_acc == 0, k_acc == n_acc - 1,
                                       [b_Sel[t], b_combhl], bbC)
                                    k_acc += 1
                        P.op("act", lambda e, bC=bC: e.copy(cbs[:], bC[:, 0:NS]), reads=[bbC], writes=[b_cbs])
                        for kc in range(KC):
                            bk, bb = nb()
                            for t in range(NT):
                                mm(bk[:, 0:CAP], hTM[:, t, kc * 128:(kc + 1) * 128], Sel[:, t, :], t == 0, t == NT - 1, [b_hTM[t], b_Sel[t]], bb)
                            P.op("act", lambda e, kc=kc, bk=bk: e.copy(hTe[:, kc, :], bk[:, 0:CAP]), reads=[bb], writes=[b_hTe[kc]])
                        P.cond_begin(flag_i[0:1, e_:e_ + 1], b_flag)
                        for b in range(NBLK):
                            bC2, bbC2 = nb()
                            for tt in range(4):
                                t = b * 4 + tt
                                dve(lambda e, tt=tt, t=t, e_=e_: e.tensor_scalar(dg[:, tt, :], ident, rt[:, 5, t, e_:e_ + 1], None, ALU.mult),
                                    [b_rt, b_consts], [b_dg[tt]])
                                mm(bC2[:, tt * 128:(tt + 1) * 128], onesf, dg[:, tt, :], True, True, [b_dg[tt], b_consts], bbC2)
                            P.op("act", lambda e, bC2=bC2, b=b: e.copy(cbt1[:, b * BLK:(b + 1) * BLK], bC2[:]), reads=[bbC2], writes=[b_cbt1])
                        P.cond_end()
                        def sp_gate_up(gi):
                            g0, gs = groups[gi]
                            ui = e_ * len(groups) + gi
                            wg_s, b_wg, wu_s, b_wu, wd_s, b_wd = wslot[ui % NSLOT]
                            hs = ui % 2
                            for c in range(gs):
                                bg, bbg = nb()
                                bu, bbu = nb()
                                for kc in range(KC):
                                    mm(bg[:, 0:CAP], wg_s[:, kc, c * 128:(c + 1) * 128], hTe[:, kc, :], kc == 0, kc == KC - 1, [b_wg, b_hTe[kc]], bbg)
                                for kc in range(KC):
                                    mm(bu[:, 0:CAP], wu_s[:, kc, c * 128:(c + 1) * 128], hTe[:, kc, :], kc == 0, kc == KC - 1, [b_wu, b_hTe[kc]], bbu)
                                act(t3[:, 0:CAP], bg[:, 0:CAP], AF.Silu, [bbg], [b_t3])
                                dve(lambda e, c=c, hs=hs, bu=bu: e.tensor_tensor(hid[:, hs, c, :], bu[:, 0:CAP], t3[:, 0:CAP], ALU.mult),
                                    [bbu, b_t3], [b_hid[hs]])

                        def sp_down(gi):
                            g0, gs = groups[gi]
                            ui = e_ * len(groups) + gi
                            wg_s, b_wg, wu_s, b_wu, wd_s, b_wd = wslot[ui % NSLOT]
                            hs = ui % 2
                            for s in range(NS):
                                for half in range(2):
                                    bo, bbo = nb()
                                    for c in range(gs):
                                        mm(bo[:], hid[:, hs, c, s * 128:(s + 1) * 128], wd_s[:, c, half * 512:(half + 1) * 512], c == 0, c == gs - 1,
                                           [b_wd, b_hid[hs]], bbo)
                                    ysl = Yacc[:, s, half * 512:(half + 1) * 512]
                                    if gi == 0:
                                        dve(lambda e, ysl=ysl, bo=bo: e.tensor_copy(ysl, bo[:]), [bbo], [b_Yacc[s]])
                                    else:
                                        dve(lambda e, ysl=ysl, bo=bo: e.tensor_tensor(ysl, bo[:], ysl, ALU.add), [bbo, b_Yacc[s]], [b_Yacc[s]])

                        def fallback_and_prefetch(gi):
                            ui = e_ * len(groups) + gi
                            P.cond_begin(flag_i[0:1, e_:e_ + 1], b_flag)
                            for b in range(NBLK):
                                gate_up(ui, b, 0, True)
                                down(ui, b, 0)
                            P.cond_end()
                            if ui + NSLOT < len(wunits):
                                load_w(ui + NSLOT)

                        for gi in range(len(groups)):
                            sp_gate_up(gi)
                            if gi >= 1:
                                sp_down(gi - 1)
                                fallback_and_prefetch(gi - 1)
                        sp_down(len(groups) - 1)
                        fallback_and_prefetch(len(groups) - 1)
                        for s in range(NS):
                            dve(lambda e, s=s: e.tensor_scalar(Ybf[:, s, :], Yacc[:, s, :], cbs[:, s:s + 1], None, ALU.mult),
                                [b_Yacc[s], b_cbs], [b_Ybf[s]])
                        for dc in range(KC):
                            for b in range(NBLK):
                                bo, bbo = nb()
                                for s in range(NS):
                                    mm(bo[:], Ybf[:, s, dc * 128:(dc + 1) * 128], SelT[:, s, b * BLK:(b + 1) * BLK], s == 0, s == NS - 1,
                                       [b_Ybf[s], b_SelT[s]], bbo)
                                add_resid(dc, b, bo, bbo)

            for b in range(NBLK):
                sl = slice(b * BLK, (b + 1) * BLK)
                rmsnorm(lambda kc: xT[:, kc, sl], [b_xT[kc][b] for kc in range(KC)], BLK, 0, 49,
                        lambda kc: xT[:, kc, sl], [b_xT[kc][b] for kc in range(KC)])
                for tt in range(4):
                    s = tt % 2
                    for half in range(2):
                        bk, bb = nb()
                        for j in range(4):
                            kc = half * 4 + j
                            P.op("pe", lambda e, kc=kc, j=j, bk=bk, b=b, tt=tt: e.transpose(
                                bk[:, j * 128:(j + 1) * 128], xT[:, kc, b * BLK + tt * 128: b * BLK + (tt + 1) * 128], ident),
                                reads=[b_xT[kc][b], b_consts], writes=[bb])
                        P.op("act", lambda e, half=half, bk=bk, s=s: e.copy(xin[:, s, half * 512:(half + 1) * 512], bk[:]),
                             reads=[bb], writes=[b_xin[s]])
                    r0 = tok0 + b * BLK + tt * 128
                    out_dmas.append(sdma(y_d[r0:r0 + 128, :], xin[:, s, :], "yo%d" % s, reads=[b_xin[s]]))

        P.emit(nc, final_waits=out_dmas)
    return nc


def _consts():
    c = np.zeros((128, 16, 128), np.float32)
    c[:, 0, :] = np.eye(128, dtype=np.float32)
    j = np.arange(128)[:, None]
    i = np.arange(128)[None, :]
    c[:, 1, :] = ((i // 64) >= (j // 64)).astype(np.float32)
    c[:, 2, :] = 1.0
    tp = np.arange(128)[:, None]
    t = np.arange(128)[None, :]
    for g, w in enumerate((2, 4, 8, 16)):
        inwin = ((t - tp) >= 0) & ((t - tp) < w)
        c[:, 3 + g, :] = inwin / float(w) - (t == tp)
        c[:, 7 + g, :] = ((t + 128 - tp) < w) / float(w)
        cnt = np.minimum(t + 1, w).astype(np.float32)
        c[:, 11 + g, :] = inwin / cnt - (t == tp)
    c[:, 15, :] = (tp < t).astype(np.float32)
    return c.reshape(128, 16 * 128)


def _host_layout(inp):
    f = lambda a: np.ascontiguousarray(np.asarray(a, dtype=np.float32))
    vecs = np.zeros((2, 128, NV), np.float32)
    bc = np.zeros((2, 128, NBC), np.float32)
    for l in range(2):
        def put(c0, v):
            v = np.asarray(v, np.float32)
            n = v.shape[0] // 128
            vecs[l, :, c0:c0 + n] = v.reshape(n, 128).T
        put(0, inp["mix_norm_g"][l])
        put(8, inp["xattn_norm_g"][l])
        put(16, inp["mem_norm_g"][l])
        put(24, inp["ffn_norm_g"][l])
        put(32, inp["conv_b"][l])
        put(35, inp["conv_ln_g"][l])
        put(38, inp["conv_ln_b"][l])
        for g in range(4):
            vecs[l, 0:64, 41 + g] = np.asarray(inp["pool_b"][l][g], np.float32)
            vecs[l, 0:64, 45 + g] = np.asarray(inp["pool_scale"][l][g * 64:(g + 1) * 64], np.float32)
        put(49, inp["final_norm_g"])
        bc[l, :, 0:384] = np.asarray(inp["sgu_ln_g"][l], np.float32)[None, :]
        bc[l, :, 384:768] = np.asarray(inp["sgu_ln_b"][l], np.float32)[None, :]
        bc[l, :, 768:1280] = np.asarray(inp["sgu_b"][l], np.float32).reshape(1, 512)
    cw = np.asarray(inp["conv_w"], np.float32)
    convw_t = np.ascontiguousarray(cw.reshape(2, 31, 3, 128).transpose(0, 3, 2, 1)).reshape(2, 128, 93)
    sw = np.asarray(inp["sgu_w"], np.float32)
    sguw_t = np.ascontiguousarray(sw.transpose(0, 3, 1, 2)).reshape(2, 128, 512)
    shared = {
        "w_in": f(inp["w_in"]), "w_out": f(inp["w_out"]),
        "xattn_wq": f(inp["xattn_wq"]), "xattn_wk": f(inp["xattn_wk"]),
        "xattn_wv": f(inp["xattn_wv"]), "xattn_wo": f(inp["xattn_wo"]),
        "ffn_wg": f(inp["ffn_wg"]), "ffn_wu": f(inp["ffn_wu"]), "ffn_wd": f(inp["ffn_wd"]),
        "router_w": f(inp["router_w"][0]),
        "moe_wg": f(inp["moe_wg"][0]), "moe_wu": f(inp["moe_wu"][0]), "moe_wd": f(inp["moe_wd"][0]),
        "vecs": vecs, "bcast": bc, "convw_t": convw_t, "sguw_t": sguw_t,
        "pool_w": f(inp["pool_w"]), "consts": _consts(),
        "iota": np.ascontiguousarray(np.broadcast_to(np.arange(CAP, dtype=np.float32)[None, :], (128, CAP))),
    }
    return shared


_NC_CACHE = {}


def kernel(**inputs):
    x = np.asarray(inputs["x"], np.float32)
    mem = np.asarray(inputs["mem"], np.float32)
    B, S, _ = x.shape
    shared = _host_layout(inputs)
    key = (S,)
    if key not in _NC_CACHE:
        _NC_CACHE[key] = build_program(S)
    nc = _NC_CACHE[key]
    in_maps = []
    for b in range(B):
        m = dict(shared)
        m["x"] = np.ascontiguousarray(x[b])
        m["mem"] = np.ascontiguousarray(mem[b])
        in_maps.append(m)
    res = run_bass_kernel_spmd(nc, in_maps, core_ids=list(range(B)))
    return np.stack([np.asarray(r["y"], np.float32) for r in res.results], axis=0)
```

```python
import contextlib
import numpy as np
import concourse.bass as bass
import concourse.mybir as mybir
from concourse.bass_utils import run_bass_kernel_spmd

F32 = mybir.dt.float32
BF16 = mybir.dt.bfloat16
AF = mybir.ActivationFunctionType
ALU = mybir.AluOpType
AX = mybir.AxisListType

ENGS = ("pe", "act", "dve", "pool", "sp")
SAME_ENGINE_SYNC = True

D = 1024
KC = 8
D_IN = 1792
DFF = 2816
NCH = DFF // 128
NE = 8
MEM = 256
EPS = 1e-6
BLK = 512
NV = 64
NBC = 384 + 384 + 512
CAP = 384
NS = CAP // 128
I32 = mybir.dt.int32


class Buf:
    __slots__ = ("name", "writers", "readers", "psum")

    def __init__(self, name="", psum=False):
        self.name = name
        self.psum = psum
        self.writers = []
        self.readers = []


class Op:
    __slots__ = ("eng", "fn", "deps", "signals", "sigval", "dma_sem", "dma_val", "is_dma", "dma_need", "cond", "regload")

    def __init__(self, eng, fn, is_dma=False):
        self.cond = None
        self.regload = None
        self.eng = eng
        self.fn = fn
        self.deps = set()
        self.signals = False
        self.sigval = None
        self.is_dma = is_dma
        self.dma_sem = None
        self.dma_val = None
        self.dma_need = {}


class Prog:
    def __init__(self):
        self.ops = {e: [] for e in ENGS}
        self.all_ops = []
        self.dma_sem_count = {}
        self.cur_cond = None
        self.n_cond = 0

    def cond_begin(self, flag_ap, b_flag):
        assert self.cur_cond is None
        self.n_cond += 1
        cid = self.n_cond
        for e in ("pe", "act", "dve"):
            o = Op(e, None)
            o.regload = flag_ap
            self._track(o, [b_flag], [])
            b_flag.readers.remove(o)
            self.ops[e].append(o)
            self.all_ops.append(o)
        self.cur_cond = cid

    def cond_end(self):
        self.cur_cond = None

    def _add_dep(self, op, d):
        if d is op:
            return
        if d.is_dma:
            op.dma_need[d.dma_sem] = self.dma_sem_count[d.dma_sem]
        else:
            op.deps.add(d)

    def _track(self, op, reads, writes):
        for b in reads:
            for w in b.writers:
                self._add_dep(op, w)
            if b.psum:
                for r in b.readers:
                    if r.eng != op.eng:
                        self._add_dep(op, r)
            b.readers.append(op)
            if len(b.readers) > 96:
                b.readers = b.readers[-96:]
        for b in writes:
            if b.readers:
                for r in b.readers:
                    self._add_dep(op, r)
                for w in b.writers:
                    self._add_dep(op, w)
                b.writers = [op]
                b.readers = []
            else:
                for w in b.writers:
                    if not (w.is_dma and op.is_dma):
                        self._add_dep(op, w)
                b.writers.append(op)
                if len(b.writers) > 96:
                    b.writers = b.writers[-96:]

    def op(self, eng, fn, reads=(), writes=()):
        o = Op(eng, fn)
        o.cond = self.cur_cond
        self._track(o, reads, writes)
        self.ops[eng].append(o)
        self.all_ops.append(o)
        return o

    def dma(self, eng, semkey, fn, reads=(), writes=()):
        assert self.cur_cond is None
        o = Op(eng, fn, is_dma=True)
        self._track(o, reads, writes)
        v = self.dma_sem_count.get(semkey, 0) + 16
        self.dma_sem_count[semkey] = v
        o.dma_sem = semkey
        o.dma_val = v
        self.ops[eng].append(o)
        self.all_ops.append(o)
        return o

    def emit(self, nc, final_waits=()):
        def skip(d, o):
            return d.eng == o.eng and (d.eng == "pe" or not SAME_ENGINE_SYNC) and not o.is_dma

        for o in self.all_ops:
            for d in o.deps:
                if not skip(d, o):
                    d.signals = True
        for e in ENGS:
            k = 0
            for o in self.ops[e]:
                if o.signals and not o.is_dma:
                    k += 1
                    o.sigval = k
        es = contextlib.ExitStack()
        with es:
            esem = {e: es.enter_context(nc.semaphore("s_" + e)) for e in ENGS}
            dsem = {k: es.enter_context(nc.semaphore("d_%s" % (k,))) for k in self.dma_sem_count}
            block = es.enter_context(nc.Block())
            prog = self

            def emit_one(e, engine, o, waited):
                need = {}
                for sk, val in o.dma_need.items():
                    need[("d", sk)] = val
                for d in o.deps:
                    if skip(d, o):
                        continue
                    key = ("e", d.eng)
                    if d.sigval > need.get(key, 0):
                        need[key] = d.sigval
                for key, val in need.items():
                    if waited.get(key, 0) >= val:
                        continue
                    waited[key] = val
                    engine.wait_ge(dsem[key[1]] if key[0] == "d" else esem[key[1]], val)
                if o.regload is not None:
                    ins = engine.load(creg[e], o.regload)
                    if o.signals:
                        ins.then_inc(esem[e], 1)
                    return
                ins = o.fn(engine)
                if o.is_dma:
                    ins.then_inc(dsem[o.dma_sem], 16)
                elif o.signals:
                    ins.then_inc(esem[e], 1)

            creg = {}

            def run(e, engine):
                waited = {}
                if e in ("pe", "act", "dve") and prog.n_cond:
                    creg[e] = engine.alloc_register("cflag_" + e)
                lst = prog.ops[e]
                i = 0
                while i < len(lst):
                    o = lst[i]
                    if o.cond is None:
                        emit_one(e, engine, o, waited)
                        i += 1
                        continue
                    j = i
                    while j < len(lst) and lst[j].cond == o.cond:
                        j += 1
                    grp = lst[i:j]
                    nsig = sum(1 for g in grp if g.signals)
                    w2 = dict(waited)
                    with engine.If_ne(creg[e], 0):
                        for g in grp:
                            emit_one(e, engine, g, w2)
                    if nsig:
                        with engine.Else():
                            engine.drain().then_inc(esem[e], nsig)
                    i = j
                if e == "sp":
                    done = set()
                    for o in final_waits:
                        if o.dma_sem not in done:
                            done.add(o.dma_sem)
                            engine.wait_ge(dsem[o.dma_sem], prog.dma_sem_count[o.dma_sem])

            @block.tensor
            def _(eng):
                run("pe", eng)

            @block.scalar
            def _(eng):
                run("act", eng)

            @block.vector
            def _(eng):
                run("dve", eng)

            @block.gpsimd
            def _(eng):
                run("pool", eng)

            @block.sync
            def _(eng):
                run("sp", eng)


def build_program(NTOK, NBLK=2, n_layers=2, G=4, NSLOT=2):
    PT = NBLK * BLK
    assert NTOK % PT == 0
    NPASS = NTOK // PT
    nc = bass.Bass("TRN2", target_bir_lowering=False)

    def din(name, shape):
        return nc.dram_tensor(name, list(shape), F32, kind="ExternalInput").ap()

    x_d = din("x", [NTOK, D])
    mem_d = din("mem", [MEM, D])
    w_in_d = din("w_in", [2, D, D_IN])
    w_out_d = din("w_out", [2, D, D])
    wq_d = din("xattn_wq", [2, D, D])
    wk_d = din("xattn_wk", [2, D, D])
    wv_d = din("xattn_wv", [2, D, D])
    wo_d = din("xattn_wo", [2, D, D])
    fwg_d = din("ffn_wg", [1, D, DFF])
    fwu_d = din("ffn_wu", [1, D, DFF])
    fwd_d = din("ffn_wd", [1, DFF, D])
    rw_d = din("router_w", [D, NE])
    mwg_d = din("moe_wg", [NE, D, DFF])
    mwu_d = din("moe_wu", [NE, D, DFF])
    mwd_d = din("moe_wd", [NE, DFF, D])
    vecs_d = din("vecs", [2, 128, NV])
    bc_d = din("bcast", [2, 128, NBC])
    cw_d = din("convw_t", [2, 128, 3 * 31])
    sguwt_d = din("sguw_t", [2, 128, 4 * 128])
    poolw_d = din("pool_w", [2, 4, 64, 64])
    consts_d = din("consts", [128, 128 * 16])
    iota_d = din("iota", [128, CAP])
    y_d = nc.dram_tensor("y", [NTOK, D], F32, kind="ExternalOutput").ap()

    P = Prog()
    es = contextlib.ExitStack()
    with es:
        def sb(name, shape, dt):
            return es.enter_context(nc.sbuf_tensor(name, list(shape), dt))

        xT = sb("xT", [128, KC, PT], F32)
        b_xT = [[Buf("xT%d_%d" % (k, b)) for b in range(NBLK)] for k in range(KC)]
        consts = sb("consts_sb", [128, 3, 128], F32)
        b_consts = Buf("consts")
        scr = sb("scr", [128, 4], F32)
        b_scr = Buf("scr")
        identb = sb("identb", [128, 128], BF16)
        onesb = sb("onesb", [128, 128], BF16)
        ptm = sb("ptm", [128, 12, 128], BF16)
        trib = sb("trib", [128, 128], BF16)
        iota = sb("iota_sb", [128, CAP], F32)
        b_iota = Buf("iota")
        b_cb16 = Buf("constsb16")
        vecs = sb("vecs_sb", [128, 2, NV], F32)
        b_vecs = Buf("vecs")
        bct = sb("bct", [128, NBC], F32)
        b_bct = Buf("bct")
        tail = sb("tail", [128, 2, 3, 30], BF16)
        b_tail = [Buf("tail%d" % l) for l in range(2)]
        cprev = sb("cprev", [128, 2, 256], BF16)
        b_cprev = [Buf("cprev%d" % l) for l in range(2)]

        ident = consts[:, 0, :]
        maskT = consts[:, 1, :]
        onesf = consts[:, 2, :]

        class Arena:
            def __init__(self, name, cols, dt):
                self.t = sb(name, [128, cols], dt)
                self.cols = cols
                self.prev = []
                self.cur = []

            def new_phase(self):
                self.prev = self.cur
                self.cur = []

            def carve(self, off, shape, name, parts=128, nbuf=1):
                n = 1
                for s_ in shape[1:]:
                    n *= s_
                assert off + n <= self.cols, (name, off, n, self.cols)
                v = self.t[0:parts, off:off + n]
                if len(shape) == 3:
                    v = v.rearrange("p (a b) -> p a b", a=shape[1])
                elif len(shape) == 4:
                    v = v.rearrange("p (a b c) -> p a b c", a=shape[1], b=shape[2])
                bs = [Buf("%s%d" % (name, i)) for i in range(nbuf)]
                self.cur.extend(bs)
                return v, (bs[0] if nbuf == 1 else bs)

        WA = Arena("arena", 37504, BF16)
        TF = Arena("tmpf", 4608, F32)
        TB = Arena("tmpb", 23552, BF16)

        def new_phase():
            for a in (WA, TF, TB):
                a.new_phase()
            prev = WA.prev + TF.prev + TB.prev
            if prev:
                P.op("dve", lambda e: e.memset(scr[:, 0:1], 0.0), reads=[], writes=[b_scr] + prev)
                P.op("act", lambda e: e.copy(scr[:, 1:2], scr[:, 2:3]), reads=[], writes=[b_scr] + prev)
            return list(WA.prev)

        sq = sb("sq", [128, 2, BLK], BF16)
        b_sq = [Buf("sq0"), Buf("sq1")]
        t1 = sb("t1", [128, BLK], F32)
        t2 = sb("t2", [128, BLK], F32)
        t3 = sb("t3", [128, BLK], F32)
        t4 = sb("t4", [128, BLK], F32)
        b_t1, b_t2, b_t3, b_t4 = Buf("t1"), Buf("t2"), Buf("t3"), Buf("t4")
        st = sb("st", [128, 8, 4], F32)
        b_st = Buf("st")
        xin = sb("xin", [128, 2, D], F32)
        b_xin = [Buf("xin0"), Buf("xin1")]
        NT_ = PT // 128
        rl = sb("rl", [128, NT_, 8], F32)
        b_rl = Buf("rl")
        rt = sb("rt", [128, 6, NT_, 8], F32)
        b_rt = Buf("rt")
        rs = sb("rs", [128, 8, NT_], F32)
        b_rs = Buf("rs")
        rs2 = sb("rs2", [128, 2, 8], F32)
        b_rs2 = Buf("rs2")
        rankm = sb("rankm", [128, NT_, 8], F32)
        b_rankm = Buf("rankm")
        maskb = sb("maskb", [128, NT_, 8], BF16)
        b_maskb = Buf("maskb")
        combhl = sb("combhl", [128, 2, NT_, 8], BF16)
        b_combhl = Buf("combhl")
        cbs = sb("cbs", [128, NS], F32)
        b_cbs = Buf("cbs")
        flag_i = sb("flag_i", [128, 8], I32)
        b_flag = Buf("flag")
        dg = sb("dg", [128, 4, 128], F32)
        b_dg = [Buf("dg%d" % i) for i in range(4)]
        rwb = sb("rwb", [128, KC, NE], BF16)
        b_rwb = Buf("rwb")
        wmt = sb("wmt", [128, 4, 128], BF16)
        b_wmt = Buf("wmt")
        plw = sb("plw", [64, 4, 64], BF16)
        b_plw = Buf("plw")

        banks = [es.enter_context(nc.psum_tensor("pb%d" % i, [128, BLK], F32)) for i in range(8)]
        b_banks = [Buf("pb%d" % i, psum=True) for i in range(8)]
        bank_rr = [0]

        def nb():
            i = bank_rr[0]
            bank_rr[0] = (i + 1) % 8
            return banks[i], b_banks[i]

        def mm(out, lhsT, rhs, start, stop, reads, wbuf):
            P.op("pe", lambda e: e.matmul(out, lhsT, rhs, start=start, stop=stop), reads=reads, writes=[wbuf])

        def act(out, in_, func, reads, writes, bias=None, scale=None):
            kw = {}
            if bias is not None:
                kw["bias"] = bias
            if scale is not None:
                kw["scale"] = scale
            P.op("act", lambda e: e.activation(out, in_, func, **kw), reads=reads, writes=writes)

        def dve(fn, reads, writes):
            P.op("dve", fn, reads=reads, writes=writes)

        def wdma(out, in_, sem, wbuf, extra_w=()):
            return P.dma("pool", sem, lambda e: e.dma_start(out=out, in_=in_), writes=[wbuf] + list(extra_w))

        def sdma(out, in_, sem, reads=(), writes=()):
            return P.dma("sp", sem, lambda e: e.dma_start(out=out, in_=in_), reads=reads, writes=writes)

        sdma(consts[:], consts_d[:, 0:384].rearrange("p (a b) -> p a b", a=3), "c0", writes=[b_consts])
        sdma(vecs[:], vecs_d.rearrange("l p n -> p l n"), "c1", writes=[b_vecs])
        wdma(identb[:], consts_d[:, 0:128], "c2", b_cb16)
        wdma(onesb[:], consts_d[:, 256:384], "c2", b_cb16)
        wdma(ptm[:], consts_d[:, 384:1920].rearrange("p (a b) -> p a b", a=12), "c2", b_cb16)
        wdma(trib[:], consts_d[:, 1920:2048], "c2", b_cb16)
        sdma(iota[:], iota_d, "c0", writes=[b_iota])
        dve(lambda e: e.memset(scr[:], 0.0), [], [b_scr])
        wdma(rwb[:], rw_d.rearrange("(k p) e -> p k e", p=128), "c2", b_rwb)

        def vcol(l, c, parts=128):
            return vecs[0:parts, l, c:c + 1]

        def rmsnorm(src, b_src, N, gl, gcol, out, b_out_list, out_f32=False):
            bk, bb = nb()
            for kc in range(KC):
                s = kc % 2
                act(sq[:, s, 0:N], src(kc), AF.Square, [b_src[kc]], [b_sq[s]])
                mm(bk[:, 0:N], onesb[:], sq[:, s, 0:N], kc == 0, kc == KC - 1, [b_sq[s], b_cb16], bb)
            act(t1[:, 0:N], bk[:, 0:N], AF.Sqrt, [bb], [b_t1], bias=EPS, scale=1.0 / D)
            dve(lambda e: e.reciprocal(t2[:, 0:N], t1[:, 0:N]), [b_t1], [b_t2])
            for kc in range(KC):
                o_ap, s_ap, g_ap = out(kc), src(kc), vcol(gl, gcol + kc)
                dve(lambda e, o_ap=o_ap, s_ap=s_ap, g_ap=g_ap: e.scalar_tensor_tensor(o_ap, s_ap, g_ap, t2[:, 0:N],
                                                                                     ALU.mult, ALU.mult),
                    [b_src[kc], b_t2, b_vecs], [b_out_list[kc]])

        def add_resid(dc, b, bk, bb):
            sl = slice(b * BLK, (b + 1) * BLK)
            dve(lambda e: e.tensor_tensor(xT[:, dc, sl], bk[:], xT[:, dc, sl], ALU.add), [bb, b_xT[dc][b]], [b_xT[dc][b]])

        out_dmas = []
        for ps_i in range(NPASS):
            tok0 = ps_i * PT
            for tt in range(PT // 128):
                s = tt % 2
                b = tt // 4
                sdma(xin[:, s, :], x_d[tok0 + tt * 128: tok0 + (tt + 1) * 128, :], "xin%d" % s, writes=[b_xin[s]])
                for half in range(2):
                    bk, bb = nb()
                    for j in range(4):
                        kc = half * 4 + j
                        P.op("pe", lambda e, kc=kc, j=j, bk=bk, s=s: e.transpose(bk[:, j * 128:(j + 1) * 128],
                                                                               xin[:, s, kc * 128:(kc + 1) * 128], ident),
                             reads=[b_xin[s], b_consts], writes=[bb])
                    P.op("act", lambda e, half=half, bk=bk, tt=tt: e.copy(
                        xT[:, half * 4:(half + 1) * 4, tt * 128:(tt + 1) * 128],
                        bk[:].rearrange("p (a b) -> p a b", a=4)),
                        reads=[bb], writes=[b_xT[half * 4 + j][b] for j in range(4)])

            for l in range(n_layers):
                ov = new_phase()
                win, b_win = WA.carve(0, [128, KC, D_IN], "win")
                woA, b_woA = WA.carve(14336, [128, 3, D], "woA")
                woB, b_woB = WA.carve(14336 + 3072, [96, 4, D], "woB", parts=96)
                woC, b_woC = WA.carve(14336 + 3072 + 4096, [64, 4, D], "woC", parts=64)
                Dm, b_Dm = WA.carve(14336 + 3072 + 8192, [128, 93, 128], "Dm")
                hc, b_hc = TF.carve(0, [128, 3, BLK], "hc", nbuf=3)
                gv, b_gv = TF.carve(1536, [128, 4, 384], "gv", nbuf=4)
                gsq, b_gsq = TF.carve(3072, [128, 4, 384], "gsq")
                glu, b_glu = TB.carve(0, [128, 3, 30 + BLK], "glu", nbuf=3)
                ya, b_ya = TB.carve(1626, [128, 3, BLK], "ya", nbuf=3)
                ug, b_ug = TB.carve(3162, [128, 4, BLK], "ug", nbuf=4)
                vn, b_vn = TB.carve(5210, [128, 4, 384], "vn", nbuf=4)
                cT, b_cT = TB.carve(6746, [128, 5, 256], "cT", nbuf=5)
                yb, b_yb = TB.carve(8026, [128, 4, BLK], "yb")
                pTm, b_pTm = TB.carve(10074, [128, 4, BLK], "pTm")
                yc, b_yc = TB.carve(12122, [128, 4, BLK], "yc")
                hTb0, b_hTb0 = TB.carve(14200, [128, KC, BLK], "hTb0")
                hTb1, b_hTb1 = TB.carve(18296, [128, KC, BLK], "hTb1")
                hTbs = [(hTb0, b_hTb0), (hTb1, b_hTb1)]
                for kh in range(2):
                    wdma(win[:, kh * 4:(kh + 1) * 4, :],
                         w_in_d[l, kh * 512:(kh + 1) * 512, :].rearrange("(k p) n -> p k n", p=128), "wA", b_win, ov)
                wdma(woA, w_out_d[l, 0:384, :].rearrange("(j p) n -> p j n", p=128), "wA", b_woA, ov)
                wdma(woB, w_out_d[l, 384:768, :].rearrange("(j p) n -> p j n", p=96), "wA", b_woB, ov)
                wdma(woC, w_out_d[l, 768:1024, :].rearrange("(j p) n -> p j n", p=64), "wA", b_woC, ov)
                sdma(t3[:, 0:93], cw_d[l], "c3", writes=[b_t3])
                for idx in range(93):
                    dve(lambda e, idx=idx: e.tensor_scalar(Dm[:, idx, :], identb[:], t3[:, idx:idx + 1], None, ALU.mult),
                        [b_t3, b_cb16], [b_Dm])
                sdma(bct[:], bc_d[l], "c4", writes=[b_bct])
                sdma(t4[:, 0:512], sguwt_d[l], "c5", writes=[b_t4])
                for h in range(4):
                    dve(lambda e, h=h: e.tensor_tensor(wmt[:, h, :], t4[:, h * 128:(h + 1) * 128], maskT, ALU.mult),
                        [b_t4, b_consts], [b_wmt])
                wdma(plw[:], poolw_d[l].rearrange("g i o -> i g o"), "c6", b_plw)

                for b in range(NBLK):
                    gb = ps_i * NBLK + b
                    sl = slice(b * BLK, (b + 1) * BLK)

                    def mix_norm(bb_):
                        hb, bhb = hTbs[bb_ % 2]
                        sl_ = slice(bb_ * BLK, (bb_ + 1) * BLK)
                        rmsnorm(lambda kc: xT[:, kc, sl_], [b_xT[kc][bb_] for kc in range(KC)], BLK, l, 0,
                                lambda kc: hb[:, kc, :], [bhb] * KC)
                    if b == 0:
                        mix_norm(0)
                    hTb, b_hTb = hTbs[b % 2]
                    if gb == 0:
                        dve(lambda e: e.memset(glu[:, :, 0:30], 0.0), [], b_glu)
                    else:
                        dve(lambda e, l=l: e.tensor_copy(glu[:, :, 0:30], tail[:, l, :, :]), [b_tail[l]], b_glu)
                    for j in range(3):
                        bv, bbv = nb()
                        bg, bbg = nb()
                        for kc in range(KC):
                            mm(bv[:], win[:, kc, j * 128:(j + 1) * 128], hTb[:, kc, :], kc == 0, kc == KC - 1, [b_win, b_hTb], bbv)
                        for kc in range(KC):
                            mm(bg[:], win[:, kc, (3 + j) * 128:(4 + j) * 128], hTb[:, kc, :], kc == 0, kc == KC - 1, [b_win, b_hTb], bbg)
                        act(t3[:], bg[:], AF.Sigmoid, [bbg], [b_t3])
                        dve(lambda e, j=j, bv=bv: e.tensor_tensor(glu[:, j, 30:30 + BLK], bv[:], t3[:], ALU.mult),
                            [bbv, b_t3], [b_glu[j]])
                    dve(lambda e, l=l: e.tensor_copy(tail[:, l, :, :], glu[:, :, BLK:BLK + 30]), b_glu, [b_tail[l]])
                    bs1, bbs1 = nb()
                    bs2, bbs2 = nb()
                    for j in range(3):
                        bc_, bbc = nb()
                        for tap in range(31):
                            mm(bc_[:], Dm[:, j * 31 + tap, :], glu[:, j, tap:tap + BLK], tap == 0, tap == 30, [b_Dm, b_glu[j]], bbc)
                        act(hc[:, j, :], bc_[:], AF.Identity, [bbc, b_vecs], [b_hc[j]], bias=vcol(l, 32 + j))
                        s = j % 2
                        act(sq[:, s, :], hc[:, j, :], AF.Square, [b_hc[j]], [b_sq[s]])
                        mm(bs2[:], onesb[:], sq[:, s, :], j == 0, j == 2, [b_sq[s], b_cb16], bbs2)
                    for j in range(3):
                        dve(lambda e, j=j: e.tensor_copy(ya[:, j, :], hc[:, j, :]), [b_hc[j]], [b_ya[j]])
                        mm(bs1[:], onesb[:], ya[:, j, :], j == 0, j == 2, [b_ya[j], b_cb16], bbs1)
                    act(t1[:], bs1[:], AF.Identity, [bbs1], [b_t1], scale=1.0 / 384)
                    dve(lambda e: e.tensor_tensor(t2[:], t1[:], t1[:], ALU.mult), [b_t1], [b_t2])
                    dve(lambda e, bs2=bs2: e.scalar_tensor_tensor(t3[:], bs2[:], 1.0 / 384, t2[:], ALU.mult, ALU.subtract),
                        [bbs2, b_t2], [b_t3])
                    act(t2[:], t3[:], AF.Sqrt, [b_t3], [b_t2], bias=EPS, scale=1.0)
                    dve(lambda e: e.reciprocal(t3[:], t2[:]), [b_t2], [b_t3])
                    for h in range(4):
                        bu, bbu = nb()
                        for kc in range(KC):
                            mm(bu[0:96, :], win[:, kc, 768 + 96 * h:768 + 96 * (h + 1)], hTb[:, kc, :], kc == 0, kc == KC - 1,
                               [b_win, b_hTb], bbu)
                        act(ug[0:96, h, :], bu[0:96, :], AF.Gelu_apprx_tanh, [bbu], [b_ug[h]])
                    if gb == 0:
                        pass
                    else:
                        dve(lambda e, l=l: e.tensor_copy(cT[:, 0, :], cprev[:, l, :]), [b_cprev[l]], [b_cT[0]])
                    for tt in range(4):
                        ba, bba = nb()
                        bq, bbq = nb()
                        tsl = slice(tt * 128, (tt + 1) * 128)
                        for kc in range(KC):
                            mm(ba[:, 0:320], hTb[:, kc, tsl], win[:, kc, 1152:1472], kc == 0, kc == KC - 1, [b_win, b_hTb], bba)
                        for kc in range(KC):
                            mm(bq[:, 0:320], hTb[:, kc, tsl], win[:, kc, 1472:1792], kc == 0, kc == KC - 1, [b_win, b_hTb], bbq)
                        act(gv[:, tt, 0:320], ba[:, 0:320], AF.Gelu_apprx_tanh, [bba], [b_gv[tt]])
                        act(gv[:, tt, 320:384], bq[:, 0:64], AF.Gelu_apprx_tanh, [bbq], [b_gv[tt]])
                        dve(lambda e, tt=tt, bq=bq: e.tensor_copy(cT[:, 1 + tt, :], bq[:, 64:320]), [bbq], [b_cT[1 + tt]])
                    dve(lambda e, l=l: e.tensor_copy(cprev[:, l, :], cT[:, 4, :]), [b_cT[4]], [b_cprev[l]])
                    dve(lambda e: e.tensor_reduce(st[:, 0, :], gv[:], AX.X, ALU.add), b_gv, [b_st])
                    dve(lambda e: e.tensor_tensor(gsq[:], gv[:], gv[:], ALU.mult), b_gv, [b_gsq])
                    dve(lambda e: e.tensor_reduce(st[:, 1, :], gsq[:], AX.X, ALU.add), [b_gsq], [b_st])
                    dve(lambda e: e.tensor_scalar(st[:, 2, :], st[:, 0, :], 1.0 / 384, None, ALU.mult), [b_st], [b_st])
                    dve(lambda e: e.tensor_tensor(st[:, 3, :], st[:, 2, :], st[:, 2, :], ALU.mult), [b_st], [b_st])
                    dve(lambda e: e.scalar_tensor_tensor(st[:, 4, :], st[:, 1, :], 1.0 / 384, st[:, 3, :], ALU.mult, ALU.subtract),
                        [b_st], [b_st])
                    act(st[:, 5, :], st[:, 4, :], AF.Sqrt, [b_st], [b_st], bias=EPS, scale=1.0)
                    dve(lambda e: e.reciprocal(st[:, 6, :], st[:, 5, :]), [b_st], [b_st])
                    for tt in range(4):
                        dve(lambda e, tt=tt: e.tensor_scalar(gsq[:, tt, :], gv[:, tt, :], st[:, 2, tt:tt + 1], st[:, 6, tt:tt + 1],
                                                             ALU.subtract, ALU.mult), [b_gv[tt], b_st], [b_gsq])
                        dve(lambda e, tt=tt: e.tensor_tensor(gsq[:, tt, :], gsq[:, tt, :], bct[:, 0:384], ALU.mult), [b_gsq, b_bct], [b_gsq])
                        dve(lambda e, tt=tt: e.tensor_tensor(vn[:, tt, :], gsq[:, tt, :], bct[:, 384:768], ALU.add), [b_gsq, b_bct], [b_vn[tt]])
                    for tt in range(4):
                        tsl = slice(tt * 128, (tt + 1) * 128)
                        bS, bbS = nb()
                        for h in range(4):
                            mm(bS[0:96, h * 128:(h + 1) * 128], vn[:, tt, 96 * h:96 * (h + 1)], wmt[:, h, :], True, True,
                               [b_vn[tt], b_wmt], bbS)
                        dve(lambda e, bS=bS: e.tensor_tensor(t4[0:96, :], bS[0:96, :], bct[0:96, 768:1280], ALU.add), [bbS, b_bct], [b_t4])
                        dve(lambda e, tsl=tsl: e.tensor_tensor(yb[0:96, :, tsl], t4[0:96, :].rearrange("p (a b) -> p a b", a=4),
                                                               ug[0:96, :, tsl], ALU.mult), [b_t4] + b_ug, [b_yb])
                        bP, bbP = nb()
                        first = (gb == 0 and tt == 0)
                        for g in range(4):
                            if first:
                                mm(bP[0:64, g * 128:(g + 1) * 128], cT[:, 1 + tt, 64 * g:64 * (g + 1)], ptm[:, 8 + g, :], True, True,
                                   [b_cT[1 + tt], b_cb16], bbP)
                            else:
                                mm(bP[0:64, g * 128:(g + 1) * 128], cT[:, 1 + tt, 64 * g:64 * (g + 1)], ptm[:, g, :], True, False,
                                   [b_cT[1 + tt], b_cb16], bbP)
                                mm(bP[0:64, g * 128:(g + 1) * 128], cT[:, tt, 64 * g:64 * (g + 1)], ptm[:, 4 + g, :], False, True,
                                   [b_cT[tt], b_cb16], bbP)
                        P.op("act", lambda e, bP=bP, tsl=tsl: e.copy(pTm[0:64, :, tsl], bP[0:64, :].rearrange("p (a b) -> p a b", a=4)),
                             reads=[bbP], writes=[b_pTm])
                    for g in range(4):
                        bQ, bbQ = nb()
                        mm(bQ[0:64, :], plw[:, g, :], pTm[0:64, g, :], True, True, [b_plw, b_pTm], bbQ)
                        dve(lambda e, g=g, bQ=bQ, l=l: e.tensor_scalar(yc[0:64, g, :], bQ[0:64, :], vcol(l, 41 + g, 64), vcol(l, 45 + g, 64),
                                                                   ALU.add, ALU.mult), [bbQ, b_vecs], [b_yc])
                    for j in range(3):
                        dve(lambda e, j=j: e.tensor_tensor(t4[:], hc[:, j, :], t1[:], ALU.subtract), [b_hc[j], b_t1], [b_t4])
                        dve(lambda e, j=j: e.tensor_tensor(t4[:], t4[:], t3[:], ALU.mult), [b_t4, b_t3], [b_t4])
                        act(ya[:, j, :], t4[:], AF.Silu, [b_t4, b_vecs], [b_ya[j]], bias=vcol(l, 38 + j), scale=vcol(l, 35 + j))
                    if b + 1 < NBLK:
                        mix_norm(b + 1)
                    for dc in range(KC):
                        bo, bbo = nb()
                        dsl = slice(dc * 128, (dc + 1) * 128)
                        for j in range(3):
                            mm(bo[:], woA[:, j, dsl], ya[:, j, :], j == 0, False, [b_woA, b_ya[j]], bbo)
                        for h in range(4):
                            mm(bo[:], woB[:, h, dsl], yb[0:96, h, :], False, False, [b_woB, b_yb], bbo)
                        for g in range(4):
                            mm(bo[:], woC[:, g, dsl], yc[0:64, g, :], False, g == 3, [b_woC, b_yc], bbo)
                        add_resid(dc, b, bo, bbo)

                ov = new_phase()
                wq, b_wq = WA.carve(0, [128, KC, D], "wq")
                wo, b_wo = WA.carve(8192, [128, KC, D], "wo")
                wkv, b_wkv = WA.carve(16384, [128, KC, D], "wkv")
                kT, b_kT = WA.carve(24576, [128, KC, MEM], "kT")
                Vt, b_Vt = WA.carve(26624, [128, 2, D], "Vt")
                mT, b_mT = WA.carve(28672, [128, KC, MEM], "mT")
                memT, b_memT = TF.carve(0, [128, KC, MEM], "memT")
                qT, b_qT = TB.carve(0, [128, KC, BLK], "qT", nbuf=KC)
                aT, b_aT = TB.carve(4096, [128, KC, BLK], "aT", nbuf=KC)
                pex, b_pex = TB.carve(8192, [128, 2, BLK], "pex", nbuf=2)
                hTb0, b_hTb0 = TB.carve(9216, [128, KC, BLK], "hTb0")
                hTb1, b_hTb1 = TB.carve(13312, [128, KC, BLK], "hTb1")
                hTbs = [(hTb0, b_hTb0), (hTb1, b_hTb1)]
                wdma(wkv, wk_d[l].rearrange("(k p) n -> p k n", p=128), "wB", b_wkv, ov)
                wdma(wq, wq_d[l].rearrange("(k p) n -> p k n", p=128), "wB", b_wq, ov)
                wdma(wo, wo_d[l].rearrange("(k p) n -> p k n", p=128), "wB", b_wo, ov)
                for mt in range(2):
                    sdma(xin[:, mt, :], mem_d[mt * 128:(mt + 1) * 128, :], "xin%d" % mt, writes=[b_xin[mt]])
                    for half in range(2):
                        bk, bb = nb()
                        for j in range(4):
                            kc = half * 4 + j
                            P.op("pe", lambda e, kc=kc, j=j, bk=bk, mt=mt: e.transpose(bk[:, j * 128:(j + 1) * 128],
                                                                                     xin[:, mt, kc * 128:(kc + 1) * 128], ident),
                                 reads=[b_xin[mt], b_consts], writes=[bb])
                        P.op("act", lambda e, half=half, bk=bk, mt=mt: e.copy(
                            memT[:, half * 4:(half + 1) * 4, mt * 128:(mt + 1) * 128],
                            bk[:].rearrange("p (a b) -> p a b", a=4)), reads=[bb], writes=[b_memT])
                rmsnorm(lambda kc: memT[:, kc, :], [b_memT] * KC, MEM, l, 16, lambda kc: mT[:, kc, :], [b_mT] * KC)
                for dc in range(KC):
                    bk, bb = nb()
                    for kc in range(KC):
                        mm(bk[:, 0:MEM], wkv[:, kc, dc * 128:(dc + 1) * 128], mT[:, kc, :], kc == 0, kc == KC - 1, [b_wkv, b_mT], bb)
                    P.op("act", lambda e, dc=dc, bk=bk: e.copy(kT[:, dc, :], bk[:, 0:MEM]), reads=[bb], writes=[b_kT])
                wdma(wkv, wv_d[l].rearrange("(k p) n -> p k n", p=128), "wB", b_wkv)
                for mt in range(2):
                    for hh in range(2):
                        bk, bb = nb()
                        for kc in range(KC):
                            mm(bk[:], mT[:, kc, mt * 128:(mt + 1) * 128], wkv[:, kc, hh * 512:(hh + 1) * 512], kc == 0, kc == KC - 1,
                               [b_wkv, b_mT], bb)
                        P.op("act", lambda e, mt=mt, hh=hh, bk=bk: e.copy(Vt[:, mt, hh * 512:(hh + 1) * 512], bk[:]), reads=[bb], writes=[b_Vt])
                for b in range(NBLK):
                    sl = slice(b * BLK, (b + 1) * BLK)

                    def att_norm(bb_):
                        hb, bhb = hTbs[bb_ % 2]
                        sl_ = slice(bb_ * BLK, (bb_ + 1) * BLK)
                        rmsnorm(lambda kc: xT[:, kc, sl_], [b_xT[kc][bb_] for kc in range(KC)], BLK, l, 8,
                                lambda kc: hb[:, kc, :], [bhb] * KC)
                    if b == 0:
                        att_norm(0)
                    hTb, b_hTb = hTbs[b % 2]
                    for dc in range(KC):
                        bk, bb = nb()
                        for kc in range(KC):
                            mm(bk[:], wq[:, kc, dc * 128:(dc + 1) * 128], hTb[:, kc, :], kc == 0, kc == KC - 1, [b_wq, b_hTb], bb)
                        P.op("act", lambda e, dc=dc, bk=bk: e.copy(qT[:, dc, :], bk[:]), reads=[bb], writes=[b_qT[dc]])
                    for h in range(4):
                        for mt in range(2):
                            bk, bb = nb()
                            for c in range(2):
                                mm(bk[:], kT[:, 2 * h + c, mt * 128:(mt + 1) * 128], qT[:, 2 * h + c, :], c == 0, c == 1,
                                   [b_kT, b_qT[2 * h + c]], bb)
                            act(pex[:, mt, :], bk[:], AF.Exp, [bb], [b_pex[mt]], scale=1.0 / 16.0)
                        bd, bbd = nb()
                        for mt in range(2):
                            mm(bd[:], onesb[:], pex[:, mt, :], mt == 0, mt == 1, [b_pex[mt], b_cb16], bbd)
                        dve(lambda e, bd=bd: e.reciprocal(t1[:], bd[:]), [bbd], [b_t1])
                        for c in range(2):
                            bo, bbo = nb()
                            for mt in range(2):
                                mm(bo[:], Vt[:, mt, (2 * h + c) * 128:(2 * h + c + 1) * 128], pex[:, mt, :], mt == 0, mt == 1,
                                   [b_Vt, b_pex[mt]], bbo)
                            dve(lambda e, h=h, c=c, bo=bo: e.tensor_tensor(aT[:, 2 * h + c, :], bo[:], t1[:], ALU.mult),
                                [bbo, b_t1], [b_aT[2 * h + c]])
                    if b + 1 < NBLK:
                        att_norm(b + 1)
                    for dc in range(KC):
                        bo, bbo = nb()
                        for kc in range(KC):
                            mm(bo[:], wo[:, kc, dc * 128:(dc + 1) * 128], aT[:, kc, :], kc == 0, kc == KC - 1, [b_wo, b_aT[kc]], bbo)
                        add_resid(dc, b, bo, bbo)

                ov = new_phase()
                moe = (l % 2 == 1)
                NT = PT // 128
                hT, _bh = WA.carve(0, [128, KC, PT], "hT")
                b_hT = [[Buf("hT%d_%d" % (k, b)) for b in range(NBLK)] for k in range(KC)]
                for k in range(KC):
                    WA.cur.extend(b_hT[k])
                wslot = []
                SLOTC = 3 * KC * G * 128
                for s_ in range(NSLOT):
                    base = KC * PT + s_ * SLOTC
                    wg_s, b_wg = WA.carve(base, [128, KC, G * 128], "wg%d" % s_)
                    wu_s, b_wu = WA.carve(base + KC * G * 128, [128, KC, G * 128], "wu%d" % s_)
                    wd_s, b_wd = WA.carve(base + 2 * KC * G * 128, [128, G, D], "wd%d" % s_)
                    wslot.append((wg_s, b_wg, wu_s, b_wu, wd_s, b_wd))
                assert NSLOT * SLOTC == 24576
                if not moe:
                    hdn, b_hdn = TB.carve(0, [128, 2, G, BLK], "hdn", nbuf=2)
                else:
                    hdn, b_hdn1 = TB.carve(0, [128, 1, G, BLK], "hdn")
                    b_hdn = [b_hdn1]
                    hTe, b_hTe = WA.carve(KC * PT + 24576, [128, KC, CAP], "hTe", nbuf=KC)
                    cbt1, b_cbt1 = TB.carve(2048, [128, PT], "cbt1")
                    hTM, b_hTM = TB.carve(3072, [128, NT, D], "hTM", nbuf=NT)
                    Sel, b_Sel = TB.carve(3072 + 8192, [128, NT, CAP], "Sel", nbuf=NT)
                    SelT, b_SelT = TB.carve(3072 + 8192 + 3072, [128, NS, PT], "SelT", nbuf=NS)
                    hid, b_hid = TB.carve(3072 + 8192 + 6144, [128, 2, G, CAP], "hid", nbuf=2)
                    Ybf, b_Ybf = TB.carve(3072 + 8192 + 9216, [128, NS, D], "Ybf", nbuf=NS)
                    Yacc, b_Yacc = TF.carve(0, [128, NS, D], "Yacc", nbuf=NS)
                NEXP = NE if moe else 1
                groups = []
                c0 = 0
                while c0 < NCH:
                    gsz = min(G, NCH - c0)
                    groups.append((c0, gsz))
                    c0 += gsz
                wunits = [(e, g0, gs) for e in range(NEXP) for (g0, gs) in groups]

                def load_w(ui):
                    e, g0, gs = wunits[ui]
                    wg_s, b_wg, wu_s, b_wu, wd_s, b_wd = wslot[ui % NSLOT]
                    if moe:
                        sg, su, sd_ = mwg_d[e], mwu_d[e], mwd_d[e]
                    else:
                        sg, su, sd_ = fwg_d[0], fwu_d[0], fwd_d[0]
                    o = ov if ui < NSLOT else []
                    cs = slice(g0 * 128, (g0 + gs) * 128)
                    wdma(wg_s[:, :, 0:gs * 128], sg[:, cs].rearrange("(k p) n -> p k n", p=128), "wF%d" % (ui % NSLOT), b_wg, o)
                    wdma(wu_s[:, :, 0:gs * 128], su[:, cs].rearrange("(k p) n -> p k n", p=128), "wF%d" % (ui % NSLOT), b_wu, o)
                    wdma(wd_s[:, 0:gs, :], sd_[cs, :].rearrange("(c p) n -> p c n", p=128), "wF%d" % (ui % NSLOT), b_wd, o)

                for b in range(NBLK):
                    sl = slice(b * BLK, (b + 1) * BLK)
                    rmsnorm(lambda kc: xT[:, kc, sl], [b_xT[kc][b] for kc in range(KC)], BLK, l, 24,
                            lambda kc: hT[:, kc, sl], [b_hT[kc][b] for kc in range(KC)])
                for ui0 in range(min(NSLOT, len(wunits))):
                    load_w(ui0)

                def gate_up(ui, b, hs, use_cb):
                    e_, g0, gs = wunits[ui]
                    wg_s, b_wg, wu_s, b_wu, wd_s, b_wd = wslot[ui % NSLOT]
                    sl = slice(b * BLK, (b + 1) * BLK)
                    for c in range(gs):
                        bg, bbg = nb()
                        bu, bbu = nb()
                        for kc in range(KC):
                            mm(bg[:], wg_s[:, kc, c * 128:(c + 1) * 128], hT[:, kc, sl], kc == 0, kc == KC - 1, [b_wg, b_hT[kc][b]], bbg)
                        for kc in range(KC):
                            mm(bu[:], wu_s[:, kc, c * 128:(c + 1) * 128], hT[:, kc, sl], kc == 0, kc == KC - 1, [b_wu, b_hT[kc][b]], bbu)
                        act(t3[:], bg[:], AF.Silu, [bbg], [b_t3])
                        if use_cb:
                            dve(lambda e, bu=bu: e.tensor_tensor(t4[:], bu[:], t3[:], ALU.mult), [bbu, b_t3], [b_t4])
                            dve(lambda e, c=c, hs=hs, sl=sl, hdn=hdn: e.tensor_tensor(hdn[:, hs, c, :], t4[:], cbt1[:, sl], ALU.mult),
                                [b_t4, b_cbt1], [b_hdn[hs]])
                        else:
                            dve(lambda e, c=c, hs=hs, bu=bu, hdn=hdn: e.tensor_tensor(hdn[:, hs, c, :], bu[:], t3[:], ALU.mult),
                                [bbu, b_t3], [b_hdn[hs]])

                def down(ui, b, hs):
                    e_, g0, gs = wunits[ui]
                    wg_s, b_wg, wu_s, b_wu, wd_s, b_wd = wslot[ui % NSLOT]
                    for dc in range(KC):
                        bo, bbo = nb()
                        for c in range(gs):
                            mm(bo[:], wd_s[:, c, dc * 128:(dc + 1) * 128], hdn[:, hs, c, :], c == 0, c == gs - 1, [b_wd, b_hdn[hs]], bbo)
                        add_resid(dc, b, bo, bbo)

                if not moe:
                    units = [(ui, b) for ui in range(len(wunits)) for b in range(NBLK)]
                    for n in range(len(units)):
                        gate_up(units[n][0], units[n][1], n % 2, False)
                        if n >= 1:
                            down(units[n - 1][0], units[n - 1][1], (n - 1) % 2)
                            ui_prev, b_prev = units[n - 1]
                            if b_prev == NBLK - 1 and ui_prev + NSLOT < len(wunits):
                                load_w(ui_prev + NSLOT)
                    down(units[-1][0], units[-1][1], (len(units) - 1) % 2)
                else:
                    bR, bbR = nb()
                    for t in range(NT):
                        for kc in range(KC):
                            mm(bR[:, t * 8:(t + 1) * 8], hT[:, kc, t * 128:(t + 1) * 128], rwb[:, kc, :], kc == 0, kc == KC - 1,
                               [b_hT[kc][t // 4], b_rwb], bbR)
                    P.op("act", lambda e, bR=bR: e.copy(rl[:], bR[:, 0:NT * 8].rearrange("p (a b) -> p a b", a=NT)), reads=[bbR], writes=[b_rl])
                    for t in range(NT):
                        L = rl[:, t, :]
                        def R(i, t=t):
                            return rt[:, i, t, :]
                        def S(i, t=t):
                            return rs[:, i, t:t + 1]
                        dve(lambda e, L=L, S=S: e.tensor_reduce(S(0), L, AX.X, ALU.max), [b_rl], [b_rs])
                        dve(lambda e, L=L, S=S, R=R: e.tensor_scalar(R(0), L, S(0), -1e30, ALU.is_equal, ALU.mult), [b_rl, b_rs], [b_rt])
                        dve(lambda e, L=L, R=R: e.tensor_tensor(R(1), R(0), L, ALU.add), [b_rl, b_rt], [b_rt])
                        dve(lambda e, S=S, R=R: e.tensor_reduce(S(1), R(1), AX.X, ALU.max), [b_rt], [b_rs])
                        dve(lambda e, L=L, S=S, R=R: e.tensor_scalar(R(2), L, S(1), None, ALU.is_ge), [b_rl, b_rs], [b_rt])
                        dve(lambda e, S=S: e.tensor_scalar(S(2), S(0), -1.0, None, ALU.mult), [b_rs], [b_rs])
                        act(R(3), L, AF.Exp, [b_rl, b_rs], [b_rt], bias=S(2))
                        dve(lambda e, R=R: e.tensor_tensor(R(4), R(3), R(2), ALU.mult), [b_rt], [b_rt])
                        dve(lambda e, S=S, R=R: e.tensor_reduce(S(3), R(4), AX.X, ALU.add), [b_rt], [b_rs])
                        dve(lambda e, S=S: e.reciprocal(S(4), S(3)), [b_rs], [b_rs])
                        dve(lambda e, S=S, R=R: e.tensor_scalar(R(5), R(4), S(4), None, ALU.mult), [b_rt, b_rs], [b_rt])
                    dve(lambda e: e.tensor_copy(maskb[:], rt[:, 2, :, :]), [b_rt], [b_maskb])
                    dve(lambda e: e.tensor_copy(combhl[:, 0, :, :], rt[:, 5, :, :]), [b_rt], [b_combhl])
                    dve(lambda e: e.tensor_tensor(rt[:, 3, :, :], rt[:, 5, :, :], combhl[:, 0, :, :], ALU.subtract), [b_rt, b_combhl], [b_rt])
                    dve(lambda e: e.tensor_copy(combhl[:, 1, :, :], rt[:, 3, :, :]), [b_rt], [b_combhl])
                    bK, bbK = nb()
                    for i in range(NT):
                        for i2 in range(i + 1):
                            mm(bK[:, i * 8:(i + 1) * 8], (onesb[:] if i2 < i else trib[:]), maskb[:, i2, :], i2 == 0, i2 == i,
                               [b_maskb, b_cb16], bbK)
                    bN, bbN = nb()
                    for i2 in range(NT):
                        mm(bN[:, 0:8], onesb[:], maskb[:, i2, :], i2 == 0, i2 == NT - 1, [b_maskb, b_cb16], bbN)
                    dve(lambda e, bN=bN: e.tensor_scalar(rs2[:, 0, :], bN[:, 0:8], float(CAP), None, ALU.is_gt), [bbN], [b_rs2])
                    dve(lambda e, bN=bN: e.tensor_scalar(rs2[:, 1, :], bN[:, 0:8], float(CAP), None, ALU.is_le), [bbN], [b_rs2])
                    dve(lambda e: e.tensor_copy(flag_i[:], rs2[:, 0, :]), [b_rs2], [b_flag])
                    for t in range(NT):
                        dve(lambda e, t=t, bK=bK: e.scalar_tensor_tensor(rankm[:, t, :], bK[:, t * 8:(t + 1) * 8], 1.0, rt[:, 2, t, :],
                                                                         ALU.add, ALU.mult), [bbK, b_rt], [b_rankm])
                        dve(lambda e, t=t: e.tensor_tensor(rankm[:, t, :], rankm[:, t, :], rs2[:, 1, :], ALU.mult), [b_rankm, b_rs2], [b_rankm])
                        dve(lambda e, t=t: e.tensor_scalar(rankm[:, t, :], rankm[:, t, :], -1.0, None, ALU.add), [b_rankm], [b_rankm])
                    for t in range(NT):
                        bk, bb = nb()
                        bkb = bk[:].bitcast(BF16)
                        for kc in range(KC):
                            P.op("pe", lambda e, kc=kc, t=t, bkb=bkb: e.transpose(bkb[:, kc * 128:(kc + 1) * 128],
                                                                                 hT[:, kc, t * 128:(t + 1) * 128], identb[:]),
                                 reads=[b_hT[kc][t // 4], b_cb16], writes=[bb])
                        P.op("act", lambda e, t=t, bkb=bkb: e.copy(hTM[:, t, :], bkb), reads=[bb], writes=[b_hTM[t]])

                    for e_ in range(NE):
                        for t in range(NT):
                            dve(lambda e, t=t, e_=e_: e.tensor_scalar(Sel[:, t, :], iota[:], rankm[:, t, e_:e_ + 1], None, ALU.is_equal),
                                [b_rankm, b_iota], [b_Sel[t]])
                        for s in range(NS):
                            bk, bb = nb()
                            bkb = bk[:].bitcast(BF16)
                            for t in range(NT):
                                P.op("pe", lambda e, t=t, s=s, bkb=bkb: e.transpose(bkb[:, t * 128:(t + 1) * 128],
                                                                                   Sel[:, t, s * 128:(s + 1) * 128], identb[:]),
                                     reads=[b_Sel[t], b_cb16], writes=[bb])
                            P.op("act", lambda e, s=s, bkb=bkb: e.copy(SelT[:, s, :], bkb), reads=[bb], writes=[b_SelT[s]])
                        bC, bbC = nb()
                        for s in range(NS):
                            n_acc = 2 * NT
                            k_acc = 0
                            for t in range(NT):
                                for hl in range(2):
                                    mm(bC[:, s:s + 1], Sel[:, t, s * 128:(s + 1) * 128], combhl[:, hl, t, e_:e_ + 1], k_acc == 0, k_acc == n_acc - 1,
                                       [b_Sel[t], b_combhl], bbC)
                                    k_acc += 1
                        P.op("act", lambda e, bC=bC: e.copy(cbs[:], bC[:, 0:NS]), reads=[bbC], writes=[b_cbs])
                        for kc in range(KC):
                            bk, bb = nb()
                            for t in range(NT):
                                mm(bk[:, 0:CAP], hTM[:, t, kc * 128:(kc + 1) * 128], Sel[:, t, :], t == 0, t == NT - 1, [b_hTM[t], b_Sel[t]], bb)
                            P.op("act", lambda e, kc=kc, bk=bk: e.copy(hTe[:, kc, :], bk[:, 0:CAP]), reads=[bb], writes=[b_hTe[kc]])
                        P.cond_begin(flag_i[0:1, e_:e_ + 1], b_flag)
                        for b in range(NBLK):
                            bC2, bbC2 = nb()
                            for tt in range(4):
                                t = b * 4 + tt
                                dve(lambda e, tt=tt, t=t, e_=e_: e.tensor_scalar(dg[:, tt, :], ident, rt[:, 5, t, e_:e_ + 1], None, ALU.mult),
                                    [b_rt, b_consts], [b_dg[tt]])
                                mm(bC2[:, tt * 128:(tt + 1) * 128], onesf, dg[:, tt, :], True, True, [b_dg[tt], b_consts], bbC2)
                            P.op("act", lambda e, bC2=bC2, b=b: e.copy(cbt1[:, b * BLK:(b + 1) * BLK], bC2[:]), reads=[bbC2], writes=[b_cbt1])
                        P.cond_end()
                        def sp_gate_up(gi):
                            g0, gs = groups[gi]
                            ui = e_ * len(groups) + gi
                            wg_s, b_wg, wu_s, b_wu, wd_s, b_wd = wslot[ui % NSLOT]
                            hs = ui % 2
                            for c in range(gs):
                                bg, bbg = nb()
                                bu, bbu = nb()
                                for kc in range(KC):
                                    mm(bg[:, 0:CAP], wg_s[:, kc, c * 128:(c + 1) * 128], hTe[:, kc, :], kc == 0, kc == KC - 1, [b_wg, b_hTe[kc]], bbg)
                                for kc in range(KC):
                                    mm(bu[:, 0:CAP], wu_s[:, kc, c * 128:(c + 1) * 128], hTe[:, kc, :], kc == 0, kc == KC - 1, [b_wu, b_hTe[kc]], bbu)
                                act(t3[:, 0:CAP], bg[:, 0:CAP], AF.Silu, [bbg], [b_t3])
                                dve(lambda e, c=c, hs=hs, bu=bu: e.tensor_tensor(hid[:, hs, c, :], bu[:, 0:CAP], t3[:, 0:CAP], ALU.mult),
                                    [bbu, b_t3], [b_hid[hs]])

                        def sp_down(gi):
                            g0, gs = groups[gi]
                            ui = e_ * len(groups) + gi
                            wg_s, b_wg, wu_s, b_wu, wd_s, b_wd = wslot[ui % NSLOT]
                            hs = ui % 2
                            for s in range(NS):
                                for half in range(2):
                                    bo, bbo = nb()
                                    for c in range(gs):
                                        mm(bo[:], hid[:, hs, c, s * 128:(s + 1) * 128], wd_s[:, c, half * 512:(half + 1) * 512], c == 0, c == gs - 1,
                                           [b_wd, b_hid[hs]], bbo)
                                    ysl = Yacc[:, s, half * 512:(half + 1) * 512]
                                    if gi == 0:
                                        dve(lambda e, ysl=ysl, bo=bo: e.tensor_copy(ysl, bo[:]), [bbo], [b_Yacc[s]])
                                    else:
                                        dve(lambda e, ysl=ysl, bo=bo: e.tensor_tensor(ysl, bo[:], ysl, ALU.add), [bbo, b_Yacc[s]], [b_Yacc[s]])

                        def fallback_and_prefetch(gi):
                            ui = e_ * len(groups) + gi
                            P.cond_begin(flag_i[0:1, e_:e_ + 1], b_flag)
                            for b in range(NBLK):
                                gate_up(ui, b, 0, True)
                                down(ui, b, 0)
                            P.cond_end()
                            if ui + NSLOT < len(wunits):
                                load_w(ui + NSLOT)

                        for gi in range(len(groups)):
                            sp_gate_up(gi)
                            sp_down(gi)
                            fallback_and_prefetch(gi)
                        for s in range(NS):
                            dve(lambda e, s=s: e.tensor_scalar(Ybf[:, s, :], Yacc[:, s, :], cbs[:, s:s + 1], None, ALU.mult),
                                [b_Yacc[s], b_cbs], [b_Ybf[s]])
                        for dc in range(KC):
                            for b in range(NBLK):
                                bo, bbo = nb()
                                for s in range(NS):
                                    mm(bo[:], Ybf[:, s, dc * 128:(dc + 1) * 128], SelT[:, s, b * BLK:(b + 1) * BLK], s == 0, s == NS - 1,
                                       [b_Ybf[s], b_SelT[s]], bbo)
                                add_resid(dc, b, bo, bbo)

            for b in range(NBLK):
                sl = slice(b * BLK, (b + 1) * BLK)
                rmsnorm(lambda kc: xT[:, kc, sl], [b_xT[kc][b] for kc in range(KC)], BLK, 0, 49,
                        lambda kc: xT[:, kc, sl], [b_xT[kc][b] for kc in range(KC)])
                for tt in range(4):
                    s = tt % 2
                    for half in range(2):
                        bk, bb = nb()
                        for j in range(4):
                            kc = half * 4 + j
                            P.op("pe", lambda e, kc=kc, j=j, bk=bk, b=b, tt=tt: e.transpose(
                                bk[:, j * 128:(j + 1) * 128], xT[:, kc, b * BLK + tt * 128: b * BLK + (tt + 1) * 128], ident),
                                reads=[b_xT[kc][b], b_consts], writes=[bb])
                        P.op("act", lambda e, half=half, bk=bk, s=s: e.copy(xin[:, s, half * 512:(half + 1) * 512], bk[:]),
                             reads=[bb], writes=[b_xin[s]])
                    r0 = tok0 + b * BLK + tt * 128
                    out_dmas.append(sdma(y_d[r0:r0 + 128, :], xin[:, s, :], "yo%d" % s, reads=[b_xin[s]]))

        P.emit(nc, final_waits=out_dmas)
    return nc


def _consts():
    c = np.zeros((128, 16, 128), np.float32)
    c[:, 0, :] = np.eye(128, dtype=np.float32)
    j = np.arange(128)[:, None]
    i = np.arange(128)[None, :]
    c[:, 1, :] = ((i // 64) >= (j // 64)).astype(np.float32)
    c[:, 2, :] = 1.0
    tp = np.arange(128)[:, None]
    t = np.arange(128)[None, :]
    for g, w in enumerate((2, 4, 8, 16)):
        inwin = ((t - tp) >= 0) & ((t - tp) < w)
        c[:, 3 + g, :] = inwin / float(w) - (t == tp)
        c[:, 7 + g, :] = ((t + 128 - tp) < w) / float(w)
        cnt = np.minimum(t + 1, w).astype(np.float32)
        c[:, 11 + g, :] = inwin / cnt - (t == tp)
    c[:, 15, :] = (tp < t).astype(np.float32)
    return c.reshape(128, 16 * 128)


def _host_layout(inp):
    f = lambda a: np.ascontiguousarray(np.asarray(a, dtype=np.float32))
    vecs = np.zeros((2, 128, NV), np.float32)
    bc = np.zeros((2, 128, NBC), np.float32)
    for l in range(2):
        def put(c0, v):
            v = np.asarray(v, np.float32)
            n = v.shape[0] // 128
            vecs[l, :, c0:c0 + n] = v.reshape(n, 128).T
        put(0, inp["mix_norm_g"][l])
        put(8, inp["xattn_norm_g"][l])
        put(16, inp["mem_norm_g"][l])
        put(24, inp["ffn_norm_g"][l])
        put(32, inp["conv_b"][l])
        put(35, inp["conv_ln_g"][l])
        put(38, inp["conv_ln_b"][l])
        for g in range(4):
            vecs[l, 0:64, 41 + g] = np.asarray(inp["pool_b"][l][g], np.float32)
            vecs[l, 0:64, 45 + g] = np.asarray(inp["pool_scale"][l][g * 64:(g + 1) * 64], np.float32)
        put(49, inp["final_norm_g"])
        bc[l, :, 0:384] = np.asarray(inp["sgu_ln_g"][l], np.float32)[None, :]
        bc[l, :, 384:768] = np.asarray(inp["sgu_ln_b"][l], np.float32)[None, :]
        bc[l, :, 768:1280] = np.asarray(inp["sgu_b"][l], np.float32).reshape(1, 512)
    cw = np.asarray(inp["conv_w"], np.float32)
    convw_t = np.ascontiguousarray(cw.reshape(2, 31, 3, 128).transpose(0, 3, 2, 1)).reshape(2, 128, 93)
    sw = np.asarray(inp["sgu_w"], np.float32)
    sguw_t = np.ascontiguousarray(sw.transpose(0, 3, 1, 2)).reshape(2, 128, 512)
    shared = {
        "w_in": f(inp["w_in"]), "w_out": f(inp["w_out"]),
        "xattn_wq": f(inp["xattn_wq"]), "xattn_wk": f(inp["xattn_wk"]),
        "xattn_wv": f(inp["xattn_wv"]), "xattn_wo": f(inp["xattn_wo"]),
        "ffn_wg": f(inp["ffn_wg"]), "ffn_wu": f(inp["ffn_wu"]), "ffn_wd": f(inp["ffn_wd"]),
        "router_w": f(inp["router_w"][0]),
        "moe_wg": f(inp["moe_wg"][0]), "moe_wu": f(inp["moe_wu"][0]), "moe_wd": f(inp["moe_wd"][0]),
        "vecs": vecs, "bcast": bc, "convw_t": convw_t, "sguw_t": sguw_t,
        "pool_w": f(inp["pool_w"]), "consts": _consts(),
        "iota": np.ascontiguousarray(np.broadcast_to(np.arange(CAP, dtype=np.float32)[None, :], (128, CAP))),
    }
    return shared


_NC_CACHE = {}


def kernel(**inputs):
    x = np.asarray(inputs["x"], np.float32)
    mem = np.asarray(inputs["mem"], np.float32)
    B, S, _ = x.shape
    shared = _host_layout(inputs)
    key = (S,)
    if key not in _NC_CACHE:
        _NC_CACHE[key] = build_program(S)
    nc = _NC_CACHE[key]
    in_maps = []
    for b in range(B):
        m = dict(shared)
        m["x"] = np.ascontiguousarray(x[b])
        m["mem"] = np.ascontiguousarray(mem[b])
        in_maps.append(m)
    res = run_bass_kernel_spmd(nc, in_maps, core_ids=list(range(B)))
    return np.stack([np.asarray(r["y"], np.float32) for r in res.results], axis=0)
```
